# Optimizing a Trainium2 kernel written in Bass

```python
import math
import jax, jax.numpy as jnp
from jax import lax
import numpy as np

D_MODEL = 1024
BATCH = 8
SEQ = 2048
DEPTH = 2

MIX_WIDTH = D_MODEL
ATTN_WIDTH = MIX_WIDTH // 2
REC_WIDTH = MIX_WIDTH - ATTN_WIDTH
ATTN_HEAD_DIM = 64
ATTN_HEADS = ATTN_WIDTH // ATTN_HEAD_DIM
DILATED_PATTERNS = ((128, 1), (512, 4), (2048, 16))
ROPE_THETA = 10000.0
REC_EXPAND = 128
REC_HEADS = REC_WIDTH // REC_EXPAND
REC_HEAD_DIM = REC_WIDTH // REC_HEADS
REC_CHUNK = 64
IN_SPLIT_SIZES = (ATTN_WIDTH, ATTN_WIDTH, ATTN_WIDTH,
                  REC_HEADS * REC_EXPAND, REC_HEADS * REC_EXPAND, REC_HEADS * REC_EXPAND,
                  REC_WIDTH, REC_WIDTH)
IN_COLS = sum(IN_SPLIT_SIZES)
D_FF_DENSE = ((8 * D_MODEL // 3 + 63) // 64) * 64
N_EXPERTS = 8
TOP_K = 2
D_FF_EXPERT = 7 * D_MODEL // 2
N_DENSE = (DEPTH + 1) // 2
N_MOE = DEPTH // 2
DN_ALPHA = (2 * DEPTH) ** 0.25
DN_BETA = (8 * DEPTH) ** -0.25
LN_EPS = 1e-5
RMS_EPS = 1e-6
MASK_VALUE = -1e30
POS_OFFSET_RANGE = 4096

kernel_name = "hybrid_dilated_attn_hgrn2_moe_encoder"


def layer_norm(x, gain, bias):
    xf = x.astype(jnp.float32)
    mu = jnp.mean(xf, axis=-1, keepdims=True)
    var = jnp.mean(jnp.square(xf - mu), axis=-1, keepdims=True)
    return ((xf - mu) * lax.rsqrt(var + LN_EPS)).astype(x.dtype) * gain + bias


def rms_norm(x, gain):
    xf = x.astype(jnp.float32)
    y = xf * lax.rsqrt(jnp.mean(xf * xf, axis=-1, keepdims=True) + RMS_EPS)
    return y.astype(x.dtype) * gain


def rope(x, positions):
    half = x.shape[-1] // 2
    inv_freq = ROPE_THETA ** (-jnp.arange(half, dtype=jnp.float32) / half)
    ang = positions.astype(jnp.float32)[..., None] * inv_freq
    cos = jnp.cos(ang)[:, :, None, :]
    sin = jnp.sin(ang)[:, :, None, :]
    xf = x.astype(jnp.float32)
    x1, x2 = xf[..., :half], xf[..., half:]
    return jnp.concatenate([x1 * cos - x2 * sin, x2 * cos + x1 * sin], axis=-1)


def split_columns(proj):
    parts, start = [], 0
    for size in IN_SPLIT_SIZES:
        parts.append(proj[..., start:start + size])
        start += size
    return parts


def dilated_window_attention(q, k, v, dilation, radius):
    B, H, S, dh = q.shape
    L = S // dilation
    blk = radius
    nb = -(-L // blk)
    Lp = nb * blk

    def to_residue(t):
        t = t.reshape(B, H, L, dilation, dh).transpose(0, 1, 3, 2, 4)
        return jnp.pad(t, ((0, 0), (0, 0), (0, 0), (0, Lp - L), (0, 0)))

    def key_windows(t):
        t = jnp.pad(to_residue(t), ((0, 0), (0, 0), (0, 0), (blk, blk), (0, 0)))
        t = t.reshape(B, H, dilation, nb + 2, blk, dh)
        return jnp.concatenate([t[:, :, :, :-2], t[:, :, :, 1:-1], t[:, :, :, 2:]], axis=4)

    qb = to_residue(q).reshape(B, H, dilation, nb, blk, dh)
    kw = key_windows(k)
    vw = key_windows(v)
    scores = jnp.einsum('bhrnqd,bhrnkd->bhrnqk', qb, kw)
    q_idx = jnp.arange(nb)[:, None, None] * blk + jnp.arange(blk)[None, :, None]
    k_idx = (jnp.arange(nb)[:, None, None] - 1) * blk + jnp.arange(3 * blk)[None, None, :]
    valid = (jnp.abs(k_idx - q_idx) <= radius) & (k_idx >= 0) & (k_idx < L)
    scores = jnp.where(valid, scores, MASK_VALUE)
    m = jnp.max(scores, axis=-1, keepdims=True)
    p = jnp.exp(scores - m)
    denom = jnp.sum(p, axis=-1, keepdims=True)
    out = jnp.einsum('bhrnqk,bhrnkd->bhrnqd', p, vw) / denom
    lse = (m + jnp.log(denom))[..., 0]
    out = out.reshape(B, H, dilation, Lp, dh)[:, :, :, :L].transpose(0, 1, 3, 2, 4).reshape(B, H, S, dh)
    lse = lse.reshape(B, H, dilation, Lp)[:, :, :, :L].transpose(0, 1, 3, 2).reshape(B, H, S)
    return out, lse


def dilated_mixture_attention(q, k, v):
    outs, lses = [], []
    for window, dilation in DILATED_PATTERNS:
        o, l = dilated_window_attention(q, k, v, dilation, window // (2 * dilation))
        outs.append(o)
        lses.append(l)
    w = jax.nn.softmax(jnp.stack(lses, axis=0), axis=0)
    return jnp.einsum('pbhs,pbhsd->bhsd', w, jnp.stack(outs, axis=0))


def gla_chunk_scan(q, k, v, log_f):
    B, H, S, dk = q.shape
    dv = v.shape[-1]
    C = REC_CHUNK
    nc = S // C
    causal = jnp.tril(jnp.ones((C, C), dtype=bool))[:, :, None]

    def split(t):
        return t.reshape(B, H, nc, C, t.shape[-1]).transpose(2, 0, 1, 3, 4)

    def step(state, inp):
        qc, kc, vc, gc = inp
        b = jnp.cumsum(gc, axis=2)
        inter = jnp.einsum('bhtc,bhcv->bhtv', qc * jnp.exp(b), state)
        diff = b[:, :, :, None, :] - b[:, :, None, :, :]
        decay = jnp.exp(jnp.where(causal, diff, -jnp.inf))
        attn = jnp.einsum('bhtc,bhsc,bhtsc->bhts', qc, kc, decay)
        out = inter + jnp.einsum('bhts,bhsv->bhtv', attn, vc)
        b_last = b[:, :, -1, :]
        state = (jnp.exp(b_last)[..., None] * state
                 + jnp.einsum('bhsc,bhsv->bhcv', kc * jnp.exp(b_last[:, :, None, :] - b), vc))
        return state, out

    state0 = jnp.zeros((B, H, dk, dv), jnp.float32)
    _, out = lax.scan(step, state0, (split(q), split(k), split(v), split(log_f)))
    return out.transpose(1, 2, 0, 3, 4).reshape(B, H, S, dv)


def hgrn2_direction(q, f_logit, v, lb):
    lb = lb[None, :, None, :]
    log_f = jnp.logaddexp(jnp.log(lb), jnp.log1p(-lb) + jax.nn.log_sigmoid(f_logit))
    k = (1.0 - lb) * jax.nn.sigmoid(-f_logit)
    return gla_chunk_scan(q, k, v, log_f)


def lower_bounds(lb_logits):
    p = jax.nn.softmax(lb_logits.astype(jnp.float32), axis=0)
    cum = jnp.cumsum(p, axis=0)
    return cum - cum[0:1]


def hybrid_mixer(u, positions, w_in, w_out, attn_gain, rec_gain, lb):
    B, S, _ = u.shape
    proj = u @ w_in
    aq, ak, av, rq, rf_fwd, rf_bwd, ri, rg = split_columns(proj)

    q = rope(aq.reshape(B, S, ATTN_HEADS, ATTN_HEAD_DIM), positions) * (ATTN_HEAD_DIM ** -0.5)
    k = rope(ak.reshape(B, S, ATTN_HEADS, ATTN_HEAD_DIM), positions)
    v = av.reshape(B, S, ATTN_HEADS, ATTN_HEAD_DIM).astype(jnp.float32)
    q, k, v = (t.transpose(0, 2, 1, 3) for t in (q, k, v))
    attn = dilated_mixture_attention(q, k, v)
    attn = attn.transpose(0, 2, 1, 3).reshape(B, S, ATTN_WIDTH)
    attn = rms_norm(attn, attn_gain).astype(u.dtype)

    def rec_heads(t, d):
        return t.reshape(B, S, REC_HEADS, d).transpose(0, 2, 1, 3).astype(jnp.float32)

    def flip(t):
        return jnp.flip(t, axis=2)

    lb = lb.reshape(2, REC_HEADS, REC_EXPAND)
    rq_h = rec_heads(jax.nn.silu(rq), REC_EXPAND)
    ri_h = rec_heads(ri, REC_HEAD_DIM)
    o_fwd = hgrn2_direction(rq_h, rec_heads(rf_fwd, REC_EXPAND), ri_h, lb[0])
    o_bwd = flip(hgrn2_direction(flip(rq_h), flip(rec_heads(rf_bwd, REC_EXPAND)), flip(ri_h), lb[1]))
    rec = (o_fwd + o_bwd).transpose(0, 2, 1, 3)
    rec = rms_norm(rec, rec_gain.reshape(REC_HEADS, REC_HEAD_DIM)).reshape(B, S, REC_WIDTH)
    rec = rec.astype(u.dtype) * jax.nn.sigmoid(rg)

    return jnp.concatenate([attn, rec], axis=-1) @ w_out


def swiglu(u, w_gate, w_up, w_down):
    return (jax.nn.silu(u @ w_gate) * (u @ w_up)) @ w_down


def moe_swiglu(u, w_router, w_gate, w_up, w_down):
    B, S, D = u.shape
    t = u.reshape(B * S, D)
    logits = (t @ w_router).astype(jnp.float32)
    top_vals, top_idx = lax.top_k(logits, TOP_K)
    top_w = jax.nn.softmax(top_vals, axis=-1)
    combine = jnp.einsum('tk,tke->te', top_w, jax.nn.one_hot(top_idx, N_EXPERTS, dtype=jnp.float32))
    combine = combine.astype(u.dtype)
    out = jnp.zeros_like(t)
    for e in range(N_EXPERTS):
        out = out + combine[:, e:e + 1] * swiglu(t, w_gate[e], w_up[e], w_down[e])
    return out.reshape(B, S, D)


def ada_modulation(c_act, w_ada, b_ada):
    ada = (c_act @ w_ada + b_ada)[:, None, :]
    return ada[..., :D_MODEL], ada[..., D_MODEL:2 * D_MODEL], ada[..., 2 * D_MODEL:]


def setup_inputs(seed: int = 0) -> dict:
    key = jax.random.key(seed)
    ks = jax.random.split(key, 20)

    def nrm(k, shape, s):
        return jax.random.normal(k, shape, jnp.float32) * s

    x = nrm(ks[0], (BATCH, SEQ, D_MODEL), 1.0)
    c = nrm(ks[1], (BATCH, D_MODEL), 1.0)
    positions = (jax.random.randint(ks[2], (BATCH, 1), 0, POS_OFFSET_RANGE, jnp.int32)
                 + jnp.arange(SEQ, dtype=jnp.int32)[None, :])
    w_in = nrm(ks[3], (DEPTH, D_MODEL, IN_COLS), D_MODEL ** -0.5)
    w_out = nrm(ks[4], (DEPTH, MIX_WIDTH, D_MODEL), MIX_WIDTH ** -0.5 * DN_BETA)
    attn_norm_gain = 1.0 + nrm(ks[5], (DEPTH, ATTN_WIDTH), 0.02)
    rec_norm_gain = 1.0 + nrm(ks[6], (DEPTH, REC_WIDTH), 0.02)
    rec_lb_logits = nrm(ks[7], (DEPTH, 2, REC_WIDTH), 0.5)
    ada_w = nrm(ks[8], (DEPTH, 2, D_MODEL, 3 * D_MODEL), 0.01)
    ada_b = nrm(ks[9], (DEPTH, 2, 3 * D_MODEL), 0.02)
    ln_gain = 1.0 + nrm(ks[10], (DEPTH, 2, D_MODEL), 0.02)
    ln_bias = nrm(ks[11], (DEPTH, 2, D_MODEL), 0.02)
    ffn_w_gate = nrm(ks[12], (N_DENSE, D_MODEL, D_FF_DENSE), D_MODEL ** -0.5)
    ffn_w_up = nrm(ks[13], (N_DENSE, D_MODEL, D_FF_DENSE), D_MODEL ** -0.5)
    ffn_w_down = nrm(ks[14], (N_DENSE, D_FF_DENSE, D_MODEL), D_FF_DENSE ** -0.5 * DN_BETA)
    moe_router = nrm(ks[15], (N_MOE, D_MODEL, N_EXPERTS), D_MODEL ** -0.5)
    moe_w_gate = nrm(ks[16], (N_MOE, N_EXPERTS, D_MODEL, D_FF_EXPERT), D_MODEL ** -0.5)
    moe_w_up = nrm(ks[17], (N_MOE, N_EXPERTS, D_MODEL, D_FF_EXPERT), D_MODEL ** -0.5)
    moe_w_down = nrm(ks[18], (N_MOE, N_EXPERTS, D_FF_EXPERT, D_MODEL), D_FF_EXPERT ** -0.5 * DN_BETA)
    return {"x": x, "c": c, "positions": positions, "w_in": w_in, "w_out": w_out,
            "attn_norm_gain": attn_norm_gain, "rec_norm_gain": rec_norm_gain,
            "rec_lb_logits": rec_lb_logits, "ada_w": ada_w, "ada_b": ada_b,
            "ln_gain": ln_gain, "ln_bias": ln_bias,
            "ffn_w_gate": ffn_w_gate, "ffn_w_up": ffn_w_up, "ffn_w_down": ffn_w_down,
            "moe_router": moe_router, "moe_w_gate": moe_w_gate, "moe_w_up": moe_w_up,
            "moe_w_down": moe_w_down}


def reference(x, c, positions, w_in, w_out, attn_norm_gain, rec_norm_gain, rec_lb_logits,
              ada_w, ada_b, ln_gain, ln_bias, ffn_w_gate, ffn_w_up, ffn_w_down,
              moe_router, moe_w_gate, moe_w_up, moe_w_down):
    lb_all = lower_bounds(rec_lb_logits)
    c_act = jax.nn.silu(c)
    for layer in range(DEPTH):
        shift, scale, gate = ada_modulation(c_act, ada_w[layer, 0], ada_b[layer, 0])
        u = x * (1.0 + scale) + shift
        y = hybrid_mixer(u, positions, w_in[layer], w_out[layer],
                         attn_norm_gain[layer], rec_norm_gain[layer], lb_all[layer])
        x = layer_norm(DN_ALPHA * x + (1.0 + gate) * y, ln_gain[layer, 0], ln_bias[layer, 0])
        shift, scale, gate = ada_modulation(c_act, ada_w[layer, 1], ada_b[layer, 1])
        u = x * (1.0 + scale) + shift
        j = layer // 2
        if layer % 2 == 0:
            y = swiglu(u, ffn_w_gate[j], ffn_w_up[j], ffn_w_down[j])
        else:
            y = moe_swiglu(u, moe_router[j], moe_w_gate[j], moe_w_up[j], moe_w_down[j])
        x = layer_norm(DN_ALPHA * x + (1.0 + gate) * y, ln_gain[layer, 1], ln_bias[layer, 1])
    return x
```

```python
import math
from contextlib import ExitStack
import numpy as np
import concourse.bass as bass
import concourse.mybir as mybir
from concourse.bass_utils import run_bass_kernel_spmd

F32 = mybir.dt.float32
BF16 = mybir.dt.bfloat16
I32 = mybir.dt.int32
AF = mybir.ActivationFunctionType
ALU = mybir.AluOpType
AX = mybir.AxisListType

S = 2048
D = 1024
NT = 16
DFF = 2752
DFE = 3584
NE = 8
ALPHA = 4.0 ** 0.25
LN_EPS = 1e-5
RMS_EPS = 1e-6
C0 = 1408
STRW = 2944
TWO_PI = 2.0 * math.pi
CW1 = 6.28125
CW2 = TWO_PI - 6.28125
PI_SAFE = 3.1415925
KSH = 30.0


class Eng:
    def __init__(self, name, h, sem):
        self.name = name
        self.h = h
        self.sem = sem
        self.count = 0
        self.waited = {}

    def wait(self, ev):
        sem, val, _ = ev
        key = id(sem)
        if self.waited.get(key, 0) >= val:
            return
        self.h.wait_ge(sem, val)
        self.waited[key] = val


class KB:
    def __init__(self):
        self.nc = bass.Bass("TRN2", target_bir_lowering=False)
        self.es = ExitStack()
        nc = self.nc
        self.E = {}
        for name, h in [("pe", nc.tensor), ("act", nc.scalar), ("dve", nc.vector),
                        ("pool", nc.gpsimd), ("sp", nc.sync)]:
            sem = self.es.enter_context(nc.semaphore("s_" + name))
            self.E[name] = Eng(name, h, sem)
        self.trk = {}
        self.slots = {}
        for q, n in (("sp", 12), ("pool", 12), ("act", 4)):
            self.slots[q] = [[self.es.enter_context(nc.semaphore("d_%s%d" % (q, i))), 0] for i in range(n)]
        self.slot_rr = {"sp": 0, "pool": 0, "act": 0}
        self.out_events = []
        self.ps = [self.es.enter_context(nc.psum_tensor("ps%d" % i, [128, 512], F32)) for i in range(8)]
        self.dbg_outs = {}

    def _deps(self, r, w, engname):
        deps = []
        for k in r:
            t = self.trk.get(k)
            if t and t[0] is not None:
                deps.append(t[0])
        for k in w:
            t = self.trk.get(k)
            if t:
                if t[0] is not None:
                    deps.append(t[0])
                for en, ev in t[1].items():
                    if en != engname:
                        deps.append(ev)
        return deps

    def _update(self, r, w, ev, engname):
        for k in r:
            t = self.trk.setdefault(k, [None, {}])
            t[1][engname] = ev
        for k in w:
            self.trk[k] = [ev, {}]

    def op(self, eng, fn, r=(), w=()):
        e = self.E[eng]
        for d in self._deps(r, w, eng):
            if eng == "pe" and d[2] == "pe":
                continue
            e.wait(d)
        ins = fn(e.h)
        e.count += 1
        ins.then_inc(e.sem, 1)
        ev = (e.sem, e.count, eng)
        self._update(r, w, ev, eng)
        return ev

    def dma(self, q, out, in_, r=(), w=(), **kw):
        e = self.E[q]
        slots = self.slots[q]
        i = self.slot_rr[q]
        self.slot_rr[q] = (i + 1) % len(slots)
        slot = slots[i]
        name = "dma_%s%d" % (q, i)
        for d in self._deps(r, w, name):
            e.wait(d)
        if slot[1] > 0:
            e.wait((slot[0], slot[1], name))
        ins = e.h.dma_start(out=out, in_=in_, **kw)
        slot[1] += 16
        ins.then_inc(slot[0], 16)
        ev = (slot[0], slot[1], name)
        self._update(r, w, ev, name)
        return ev

    def barrier(self):
        evs = [(e.sem, e.count, e.name) for e in self.E.values() if e.count > 0]
        for q, sl in self.slots.items():
            for i, s in enumerate(sl):
                if s[1] > 0:
                    evs.append((s[0], s[1], "dma_%s%d" % (q, i)))
        for e in self.E.values():
            for ev in evs:
                if ev[2] == e.name:
                    continue
                e.wait(ev)
        self.trk = {}

    def sb(self, es, name, shape, dt):
        self.uid = getattr(self, "uid", 0) + 1
        return es.enter_context(self.nc.sbuf_tensor("%s_%d" % (name, self.uid), list(shape), dt))

    def dbg(self, name, ap, shape, dt=F32, r=()):
        if self.debug is None or name not in self.debug:
            return
        t = self.nc.dram_tensor("dbg_" + name, list(shape), dt, kind="ExternalOutput").ap()
        ev = self.dma("sp", t, ap, r=r)
        self.out_events.append(ev)
        self.dbg_outs[name] = "dbg_" + name


def build(debug=None, stop_after=None, small_moe=False):
    k = KB()
    k.debug = debug
    nc = k.nc
    es = k.es
    op = k.op
    dma = k.dma
    ps = k.ps

    def din(name, shape, dt=F32):
        return nc.dram_tensor(name, list(shape), dt, kind="ExternalInput").ap()

    x_in = din("x", [S, D])
    cT_in = din("cT", [128, 8])
    pos_in = din("pos", [1, S], I32)
    wb_in = [din("wb0", [40, 128, 1024]), din("wb1", [40, 128, 1024])]
    wo_in = din("wo", [2, D, D])
    ag_in = din("ag", [2, 512])
    rgain_in = din("rgain", [2, 512])
    lbl_in = din("lbl", [16, 128])
    adaw_in = din("adaw", [4, D, 3 * D])
    adab_in = din("adab", [4, 3 * D])
    lng_in = din("lng", [4, D])
    lnb_in = din("lnb", [4, D])
    fwg_in = din("fwg", [D, DFF])
    fwu_in = din("fwu", [D, DFF])
    fwd_in = din("fwd", [DFF, D])
    mr_in = din("mr", [D, NE])
    if small_moe:
        mwg_in = din("mwg", [1, 128, 128]); mwu_in = din("mwu", [1, 128, 128]); mwd_in = din("mwd", [1, 128, 128])
    else:
        mwg_in = din("mwg", [NE, D, DFE])
        mwu_in = din("mwu", [NE, D, DFE])
        mwd_in = din("mwd", [NE, DFE, D])
    y_out = nc.dram_tensor("y", [S, D], F32, kind="ExternalOutput").ap()
    xsA = nc.dram_tensor("xsA", [S, D], F32, kind="Internal").ap()
    xsB = nc.dram_tensor("xsB", [S, D], F32, kind="Internal").ap()

    sb = k.sb
    ones_f = sb(es, "ones_f", [128, 512], F32)
    ident_f = sb(es, "ident_f", [128, 128], F32)
    ident_b = sb(es, "ident_b", [128, 128], BF16)
    strip = sb(es, "strip", [128, STRW], BF16)
    mu = sb(es, "mu", [128, 64], F32)
    ml = sb(es, "ml", [128, 64], F32)
    mui = sb(es, "mui", [128, 8, 64], mybir.dt.uint8)
    mli = sb(es, "mli", [128, 8, 64], mybir.dt.uint8)
    rope_tab = nc.dram_tensor("rope_tab", [2, 128, S], F32, kind="Internal").ap()
    lb = sb(es, "lb", [128, 16], F32)
    oml = sb(es, "oml", [128, 16], F32)
    adac = sb(es, "adac", [128, 4, 16], F32)
    grow_d = nc.dram_tensor("grow_d", [4, 128, D], F32, kind="Internal").ap()
    cact = sb(es, "cact", [128, 8], F32)
    one11 = ones_f[0:1, 0:1]
    eps_rms = sb(es, "eps_rms", [128, 1], F32)
    eps_ln = sb(es, "eps_ln", [128, 1], F32)
    op("dve", lambda h: h.memset(eps_rms[:], RMS_EPS), w=["eps_rms"])
    op("dve", lambda h: h.memset(eps_ln[:], LN_EPS), w=["eps_ln"])
    nsh = sb(es, "nsh", [128, 1], F32)
    zero1 = sb(es, "zero1", [128, 1], F32)
    op("dve", lambda h: h.memset(nsh[:], -KSH), w=["nsh"])
    op("dve", lambda h: h.memset(zero1[:], 0.0), w=["zero1"])

    op("dve", lambda h: h.memset(ones_f[:], 1.0), w=["ones_f"])
    op("pool", lambda h: h.affine_select(out=ident_f[:], in_=ones_f[:, 0:128], pattern=[[-1, 128]],
                                          compare_op=ALU.is_equal, fill=0.0, base=0, channel_multiplier=1),
       r=["ones_f"], w=["ident_f"])
    op("dve", lambda h: h.tensor_copy(out=ident_b[:], in_=ident_f[:]), r=["ident_f"], w=["ident_b"])
    for half in range(2):
        sl = slice(half * 64, half * 64 + 64)
        op("pool", lambda h: h.affine_select(out=mu[sl, :], in_=ones_f[sl, 0:64], pattern=[[1, 64]],
                                              compare_op=ALU.is_ge, fill=0.0, base=0, channel_multiplier=-1),
           r=["ones_f"], w=["mu"])
        op("pool", lambda h: h.affine_select(out=ml[sl, :], in_=ones_f[sl, 0:64], pattern=[[-1, 64]],
                                              compare_op=ALU.is_ge, fill=0.0, base=0, channel_multiplier=1),
           r=["ones_f"], w=["ml"])

    for t8 in range(8):
        op("dve", lambda h: h.tensor_copy(out=mui[:, t8, :], in_=mu[:]), r=["mu"], w=["mui"])
        op("dve", lambda h: h.tensor_copy(out=mli[:, t8, :], in_=ml[:]), r=["ml"], w=["mli"])
    with ExitStack() as ts:
        vi = sb(ts, "vi", [128, STRW], I32)
        vj = sb(ts, "vj", [128, STRW], I32)
        vf = sb(ts, "vf", [128, STRW], F32)
        t1 = sb(ts, "t1", [128, STRW], F32)
        t2 = sb(ts, "t2", [128, STRW], F32)
        acc = sb(ts, "acc", [128, STRW], F32)
        op("pool", lambda h: h.iota(vi[:], pattern=[[-1, STRW]], base=C0, channel_multiplier=1), w=["vi"])
        op("dve", lambda h: h.tensor_copy(out=vf[:], in_=vi[:]), r=["vi"], w=["vf"])
        def inrange(dst, key, lim):
            op("dve", lambda h: h.tensor_scalar(out=dst[:], in0=vf[:], scalar1=lim, scalar2=None, op0=ALU.is_le),
               r=["vf"], w=[key])
            op("dve", lambda h: h.scalar_tensor_tensor(out=dst[:], in0=vf[:], scalar=-lim, in1=dst[:],
                                                        op0=ALU.is_ge, op1=ALU.mult), r=["vf", key], w=[key])
        inrange(acc, "acc", 64.0)
        for msk, lim in ((3, 256.0), (15, 1024.0)):
            op("dve", lambda h: h.tensor_scalar(out=vj[:], in0=vi[:], scalar1=msk, scalar2=None,
                                                 op0=ALU.bitwise_and), r=["vi"], w=["vj"])
            op("dve", lambda h: h.tensor_copy(out=t1[:], in_=vj[:]), r=["vj"], w=["t1"])
            op("dve", lambda h: h.tensor_scalar(out=t1[:], in0=t1[:], scalar1=0.0, scalar2=None,
                                                 op0=ALU.is_equal), r=["t1"], w=["t1"])
            inrange(t2, "t2", lim)
            op("dve", lambda h: h.tensor_tensor(out=t1[:], in0=t1[:], in1=t2[:], op=ALU.mult),
               r=["t1", "t2"], w=["t1"])
            op("dve", lambda h: h.tensor_tensor(out=acc[:], in0=acc[:], in1=t1[:], op=ALU.add),
               r=["acc", "t1"], w=["acc"])
        op("dve", lambda h: h.tensor_copy(out=strip[:], in_=acc[:]), r=["acc"], w=["strip"])
        k.dbg("strip", acc[:], [128, STRW], r=["acc"])
        k.barrier()

    with ExitStack() as ts:
        posi = sb(ts, "posi", [128, S], I32)
        ang = sb(ts, "ang", [128, S], F32)
        kf = sb(ts, "kf", [128, S], F32)
        ki = sb(ts, "ki", [128, S], I32)
        rr = sb(ts, "rr", [128, S], F32)
        mm = sb(ts, "mm", [128, S], F32)
        pidx = sb(ts, "pidx", [128, 1], I32)
        pj = sb(ts, "pj", [128, 1], I32)
        pf = sb(ts, "pf", [128, 1], F32)
        invf = sb(ts, "invf", [128, 1], F32)
        sgn = sb(ts, "sgn", [128, 1], F32)
        cosT = sb(ts, "cosT", [128, S], F32)
        sinT = sb(ts, "sinT", [128, S], F32)
        dma("sp", posi[:], pos_in[0:1, :].to_broadcast([128, S]), w=["posi"])
        op("pool", lambda h: h.iota(pidx[:], pattern=[[0, 1]], base=0, channel_multiplier=1), w=["pidx"])
        op("dve", lambda h: h.tensor_scalar(out=pj[:], in0=pidx[:], scalar1=31, scalar2=None, op0=ALU.bitwise_and),
           r=["pidx"], w=["pj"])
        op("dve", lambda h: h.tensor_copy(out=pf[:], in_=pj[:]), r=["pj"], w=["pf"])
        op("act", lambda h: h.activation(out=invf[:], in_=pf[:], func=AF.Exp, scale=-math.log(10000.0) / 32.0),
           r=["pf"], w=["invf"])
        op("dve", lambda h: h.tensor_scalar(out=pj[:], in0=pidx[:], scalar1=32, scalar2=None, op0=ALU.bitwise_and),
           r=["pidx", "pf"], w=["pj"])
        op("dve", lambda h: h.tensor_copy(out=pf[:], in_=pj[:]), r=["pj", "invf"], w=["pf"])
        op("dve", lambda h: h.tensor_scalar(out=sgn[:], in0=pf[:], scalar1=1.0 / 16.0, scalar2=-1.0,
                                             op0=ALU.mult, op1=ALU.add), r=["pf"], w=["sgn"])
        op("dve", lambda h: h.tensor_copy(out=ang[:], in_=posi[:]), r=["posi"], w=["ang"])
        op("dve", lambda h: h.tensor_scalar(out=ang[:], in0=ang[:], scalar1=invf[:, 0:1], scalar2=None,
                                             op0=ALU.mult), r=["ang", "invf"], w=["ang"])
        for which, dst in ((0, sinT), (1, cosT)):
            shift = 0.0 if which == 0 else math.pi / 2.0
            op("dve", lambda h: h.tensor_scalar(out=kf[:], in0=ang[:], scalar1=shift, scalar2=1.0 / TWO_PI,
                                                 op0=ALU.add, op1=ALU.mult), r=["ang"], w=["kf"])
            op("dve", lambda h: h.tensor_copy(out=ki[:], in_=kf[:]), r=["kf"], w=["ki"])
            op("dve", lambda h: h.tensor_copy(out=kf[:], in_=ki[:]), r=["ki"], w=["kf"])
            op("dve", lambda h: h.tensor_scalar(out=rr[:], in0=ang[:], scalar1=shift, scalar2=None, op0=ALU.add),
               r=["ang"], w=["rr"])
            op("dve", lambda h: h.scalar_tensor_tensor(out=rr[:], in0=kf[:], scalar=-CW1, in1=rr[:],
                                                        op0=ALU.mult, op1=ALU.add), r=["kf", "rr"], w=["rr"])
            op("dve", lambda h: h.scalar_tensor_tensor(out=rr[:], in0=kf[:], scalar=-CW2, in1=rr[:],
                                                        op0=ALU.mult, op1=ALU.add), r=["kf", "rr"], w=["rr"])
            op("dve", lambda h: h.tensor_scalar(out=mm[:], in0=rr[:], scalar1=math.pi, scalar2=None,
                                                 op0=ALU.is_gt), r=["rr"], w=["mm"])
            op("dve", lambda h: h.scalar_tensor_tensor(out=rr[:], in0=mm[:], scalar=-TWO_PI, in1=rr[:],
                                                        op0=ALU.mult, op1=ALU.add), r=["mm", "rr"], w=["rr"])
            op("dve", lambda h: h.tensor_scalar(out=mm[:], in0=rr[:], scalar1=-math.pi, scalar2=None,
                                                 op0=ALU.is_lt), r=["rr"], w=["mm"])
            op("dve", lambda h: h.scalar_tensor_tensor(out=rr[:], in0=mm[:], scalar=TWO_PI, in1=rr[:],
                                                        op0=ALU.mult, op1=ALU.add), r=["mm", "rr"], w=["rr"])
            op("dve", lambda h: h.tensor_scalar(out=rr[:], in0=rr[:], scalar1=-PI_SAFE, scalar2=PI_SAFE,
                                                 op0=ALU.max, op1=ALU.min), r=["rr"], w=["rr"])
            op("act", lambda h: h.activation(out=dst[:], in_=rr[:], func=AF.Sin), r=["rr"], w=["tab%d" % which])
        op("dve", lambda h: h.tensor_scalar(out=sinT[:], in0=sinT[:], scalar1=sgn[:, 0:1], scalar2=None,
                                             op0=ALU.mult), r=["tab0", "sgn"], w=["tab0"])
        dma("sp", rope_tab[0], cosT[:], r=["tab1"], w=["rope_tab0"])
        dma("sp", rope_tab[1], sinT[:], r=["tab0"], w=["rope_tab1"])
        k.dbg("cosT", cosT[:], [128, S], r=["tab1"])
        k.dbg("sinT", sinT[:], [128, S], r=["tab0"])
        k.barrier()

    with ExitStack() as ts:
        lrow = sb(ts, "lrow", [16, 128], F32)
        el = sb(ts, "el", [128, 16], F32)
        den = sb(ts, "den", [128, 8], F32)
        cT = sb(ts, "cT", [128, 8], F32)
        adarow = sb(ts, "adarow", [1, 3 * D], F32)
        adab = sb(ts, "adab", [1, 3 * D], F32)
        aw = [sb(ts, "aw%d" % i, [128, 8, 512], BF16) for i in range(4)]
        cact_b = sb(ts, "cact_b", [128, 8], BF16)
        grow = sb(ts, "grow", [128, 4, D], F32)
        dma("sp", lrow[:], lbl_in[:, :], w=["lrow"])
        op("pe", lambda h: h.transpose(out=ps[0][:, 0:16], in_=lrow[:], identity=ident_f[0:16, 0:16]),
           r=["lrow", "ident_f"], w=["ps0"])
        op("act", lambda h: h.activation(out=el[:], in_=ps[0][:, 0:16], func=AF.Exp), r=["ps0"], w=["el"])
        op("dve", lambda h: h.tensor_tensor(out=den[:], in0=el[:, 0:8], in1=el[:, 8:16], op=ALU.add),
           r=["el"], w=["den"])
        op("dve", lambda h: h.reciprocal(out=den[:], in_=den[:]), r=["den"], w=["den"])
        op("dve", lambda h: h.memset(lb[:, 0:8], 0.0), w=["lb"])
        op("dve", lambda h: h.tensor_tensor(out=lb[:, 8:16], in0=el[:, 8:16], in1=den[:], op=ALU.mult),
           r=["el", "den", "lb"], w=["lb"])
        op("dve", lambda h: h.tensor_scalar(out=oml[:], in0=lb[:], scalar1=-1.0, scalar2=1.0, op0=ALU.mult,
                                             op1=ALU.add), r=["lb"], w=["oml"])
        k.dbg("lb", lb[:], [128, 16], r=["lb"])
        dma("sp", cT[:], cT_in[:, :], w=["cT"])
        op("act", lambda h: h.activation(out=cact[:], in_=cT[:], func=AF.Silu), r=["cT"], w=["cact"])
        op("dve", lambda h: h.tensor_copy(out=cact_b[:], in_=cact[:]), r=["cact"], w=["cact_b"])
        n_aw = 0
        for ls in range(4):
            dma("sp", adab[:], adab_in[ls:ls + 1, :], w=["adab"])
            for cg in range(6):
                buf = aw[n_aw % 4]
                bk = "aw%d" % (n_aw % 4)
                n_aw += 1
                dma("pool", buf[:], adaw_in[ls].rearrange("(k p) f -> p k f", p=128)[:, :, cg * 512:(cg + 1) * 512],
                    w=[bk])
                for kk in range(8):
                    op("pe", lambda h: h.matmul(ps[1][0:1, :], lhsT=cact_b[:, kk:kk + 1], rhs=buf[:, kk, :],
                                                start=(kk == 0), stop=(kk == 7)),
                       r=[bk, "cact_b"], w=["ps1"])
                op("dve", lambda h: h.tensor_tensor(out=adarow[0:1, cg * 512:(cg + 1) * 512], in0=ps[1][0:1, :],
                                                     in1=adab[0:1, cg * 512:(cg + 1) * 512], op=ALU.add),
                   r=["ps1", "adab"], w=["adarow"])
            for j in range(16):
                op("pe", lambda h: h.matmul(ps[2][:, j:j + 1], lhsT=adarow[0:1, j * 128:(j + 1) * 128], rhs=one11,
                                            start=True, stop=True), r=["adarow", "ones_f"], w=["ps2"])
            op("dve", lambda h: h.tensor_copy(out=adac[:, ls, :], in_=ps[2][:, 0:16]), r=["ps2"], w=["adac"])
            op("dve", lambda h: h.tensor_scalar(out=adac[:, ls, 8:16], in0=adac[:, ls, 8:16], scalar1=1.0,
                                                 scalar2=None, op0=ALU.add), r=["adac"], w=["adac"])
            for half in range(2):
                op("pe", lambda h: h.matmul(ps[3][:, :], lhsT=ones_f[0:1, 0:128],
                                            rhs=adarow[0:1, 2048 + half * 512:2048 + (half + 1) * 512],
                                            start=True, stop=True), r=["adarow", "ones_f"], w=["ps3"])
                op("dve", lambda h: h.tensor_scalar(out=grow[:, ls, half * 512:(half + 1) * 512], in0=ps[3][:, :],
                                                     scalar1=1.0, scalar2=None, op0=ALU.add), r=["ps3"], w=["grow"])
        for ls in range(4):
            dma("sp", grow_d[ls], grow[:, ls, :], r=["grow"], w=[("grow_d", ls)])
        k.dbg("adac", adac[:], [128, 4, 16], r=["adac"])
        k.dbg("grow", grow[:], [128, 4, D], r=["grow"])
        k.barrier()

    if stop_after == "consts":
        return finish(k, y_out)
    C = dict(locals())
    for layer in range(2):
        src = x_in if layer == 0 else xsB
        if mixer(k, C, layer, src, xsA, stop=(stop_after[3:] if stop_after and stop_after.startswith("m%d_" % layer) else None)):
            return finish(k, y_out)
        k.dbg("x_mix%d" % layer, xsA, [S, D])
        if stop_after == "mix%d" % layer:
            return finish(k, y_out)
        ffn(k, C, layer, xsA, xsB if layer == 0 else y_out, is_final=(layer == 1))
        if layer == 0:
            k.dbg("x_ffn0", xsB, [S, D])
        if stop_after == "ffn%d" % layer:
            return finish(k, y_out)
    return finish(k, y_out)


def finish(k, y_out):
    e = k.E["sp"]
    for ev in k.out_events:
        e.wait(ev)
    k.es.close()
    return k


def _perm_swap():
    idx = []
    for h in range(8):
        idx += list(range(h * 64 + 32, h * 64 + 64)) + list(range(h * 64, h * 64 + 32))
    return np.array(idx)


def prep_shared(inp):
    f = lambda a: np.ascontiguousarray(np.asarray(a, dtype=np.float32))
    w_in = np.asarray(inp["w_in"], dtype=np.float32)
    p = _perm_swap()
    sh = {}
    for l in range(2):
        w = w_in[l]
        ext = np.concatenate([w[:, 0:512], w[:, 0:512][:, p], w[:, 512:1024], w[:, 512:1024][:, p],
                              w[:, 1024:]], axis=1)
        blk = ext.reshape(8, 128, 40, 128).transpose(2, 1, 0, 3).reshape(40, 128, 1024)
        sh["wb%d" % l] = np.ascontiguousarray(blk)
    sh["wo"] = f(inp["w_out"])
    sh["ag"] = f(inp["attn_norm_gain"])
    sh["rgain"] = f(inp["rec_norm_gain"])
    sh["lbl"] = f(inp["rec_lb_logits"]).reshape(16, 128)
    sh["adaw"] = f(inp["ada_w"]).reshape(4, D, 3 * D)
    sh["adab"] = f(inp["ada_b"]).reshape(4, 3 * D)
    sh["lng"] = f(inp["ln_gain"]).reshape(4, D)
    sh["lnb"] = f(inp["ln_bias"]).reshape(4, D)
    sh["fwg"] = f(inp["ffn_w_gate"])[0]
    sh["fwu"] = f(inp["ffn_w_up"])[0]
    sh["fwd"] = f(inp["ffn_w_down"])[0]
    sh["mr"] = f(inp["moe_router"])[0]
    sh["mwg"] = f(inp["moe_w_gate"])[0]
    sh["mwu"] = f(inp["moe_w_up"])[0]
    sh["mwd"] = f(inp["moe_w_down"])[0]
    return sh


def prep_core(inp, b):
    return {
        "x": np.ascontiguousarray(np.asarray(inp["x"][b], dtype=np.float32)),
        "cT": np.ascontiguousarray(np.asarray(inp["c"][b], dtype=np.float32).reshape(8, 128).T),
        "pos": np.ascontiguousarray(np.asarray(inp["positions"][b], dtype=np.int32).reshape(1, S)),
    }


def kernel(**inputs):
    sh = prep_shared(inputs)
    kb = build()
    in_maps = []
    for b in range(8):
        m = dict(sh)
        m.update(prep_core(inputs, b))
        in_maps.append(m)
    res = run_bass_kernel_spmd(kb.nc, in_maps, core_ids=list(range(8)))
    return np.stack([np.asarray(r["y"], dtype=np.float32) for r in res.results], axis=0)


class WPool:
    def __init__(self, k, es, wb, n, tag):
        self.k = k
        self.wb = wb
        self.bufs = [k.sb(es, "wblk", [128, 8, 128], BF16) for _ in range(n)]
        self.keys = ["%s_wblk%d" % (tag, i) for i in range(n)]
        self.i = 0

    def load(self, j):
        i = self.i
        self.i = (i + 1) % len(self.bufs)
        self.k.dma("pool", self.bufs[i][:], self.wb[j].rearrange("p (k c) -> p k c", c=128), w=[self.keys[i]])
        return self.bufs[i], self.keys[i]


def build_ut(k, C, UT, utag, ls, x_src=None, X=None, router=None):
    op, dma, ps, sb = k.op, k.dma, k.ps, k.sb
    adac, ident_f = C["adac"], C["ident_f"]
    with ExitStack() as ts:
        if X is None:
            xst = [sb(ts, "xst", [128, 4, D], F32) for _ in range(2)]
        for g in range(4):
            if X is None:
                buf = xst[g % 2]
                bk = "%s_xst%d" % (utag, g % 2)
                dma("sp", buf[:], x_src[g * 512:(g + 1) * 512, :].rearrange("(t p) d -> p t d", p=128), w=[bk])
                rk = [bk]
                tile = lambda t: buf[:, t, :]
            else:
                rk = [("X", 4 * g + t, hf) for t in range(4) for hf in range(2)]
                tile = lambda t: X[:, 4 * g + t, :]
            for kk in range(8):
                pb = ps[kk % 2]
                pk = "ps%d" % (kk % 2)
                for t in range(4):
                    op("pe", lambda h: h.transpose(out=pb[:, t * 128:(t + 1) * 128],
                                                   in_=tile(t)[:, kk * 128:(kk + 1) * 128], identity=ident_f[:]),
                       r=rk + ["ident_f"], w=[pk])
                op("act", lambda h: h.activation(out=UT[:, kk, g * 512:(g + 1) * 512], in_=pb[:, :], func=AF.Identity,
                                                 scale=adac[:, ls, 8 + kk:9 + kk], bias=adac[:, ls, kk:kk + 1]),
                   r=[pk, "adac"], w=[(utag, kk, g)])
                if router is not None:
                    router(g, kk, pb, pk)


def ut_keys(utag, g):
    return [(utag, kk, g) for kk in range(8)]


def mixer(k, C, layer, x_src, x_dst, stop=None):
    nc, op, dma, ps, sb = k.nc, k.op, k.dma, k.ps, k.sb
    ones_f, ident_f, ident_b, strip = C["ones_f"], C["ident_f"], C["ident_b"], C["strip"]
    wb = C["wb_in"][layer]
    ls = layer * 2
    T = "m%d" % layer
    utag = T + "UT"
    with ExitStack() as ms:
        UT = sb(ms, "UT", [128, 8, S], BF16)
        CATT = sb(ms, "CATT", [128, 8, S], BF16)
        build_ut(k, C, UT, utag, ls, x_src=x_src)
        k.dbg("UT%d" % layer, UT[:], [128, 8, S], BF16, r=[(utag, kk, g) for kk in range(8) for g in range(4)])
        if stop == "ut":
            k.barrier()
            return True
        with ExitStack() as as_:
            QT = sb(as_, "QT", [128, 4, S], BF16)
            KT = sb(as_, "KT", [128, 4, S], BF16)
            VA = sb(as_, "VA", [128, NT, 8, 65], BF16)
            WV = sb(as_, "WV", [128, 4, 8, 128], BF16)
            zer = sb(as_, "zer", [128, 512], BF16)
            AG = sb(as_, "AG", [128, 512], F32)
            ta = [sb(as_, "ta", [128, 512], F32) for _ in range(2)]
            tb_ = [sb(as_, "tb", [128, 512], F32) for _ in range(2)]
            Eb = [sb(as_, "Eb", [128, 512], BF16) for _ in range(3)]
            Em = [sb(as_, "Em", [128, 512], BF16) for _ in range(3)]
            ATT = sb(as_, "ATT", [128, 4, 512], F32)
            ATB = [sb(as_, "ATB", [128, 512], BF16) for _ in range(2)]
            junk = sb(as_, "junk", [128, 512], F32)
            ss = sb(as_, "ss", [128, 4], F32)
            rcp = sb(as_, "rcp", [128, 4], F32)
            wp = WPool(k, as_, wb, 4, T + "a")
            cosT = sb(as_, "cosT", [128, S], F32)
            sinT = sb(as_, "sinT", [128, S], F32)
            dma("sp", cosT[:], C["rope_tab"][0], w=["cosT"])
            dma("sp", sinT[:], C["rope_tab"][1], w=["sinT"])
            op("dve", lambda h: h.memset(zer[:], 0.0), w=["zer"])
            op("pool", lambda h: h.memset(VA[:], 1.0), w=["VA"])
            dma("sp", AG[:], C["ag_in"][layer:layer + 1, :].to_broadcast([128, 512]), w=["AG"])
            it = 0
            for c in range(4):
                dma("pool", WV[:, c, :, :], wb[16 + c].rearrange("p (k c) -> p k c", c=128), w=[("WV", c)])
            qk_blocks = [(dst, dname, jq + c, jqs + c, c)
                         for (dst, dname, jq, jqs) in ((QT, "QT", 0, 4), (KT, "KT", 8, 12)) for c in range(4)]
            loaded = [(wp.load(qk_blocks[0][2]), wp.load(qk_blocks[0][3]))]
            for bi_, (dst, dname, _jq, _jqs, c) in enumerate(qk_blocks):
                if True:
                    if bi_ + 1 < len(qk_blocks):
                        loaded.append((wp.load(qk_blocks[bi_ + 1][2]), wp.load(qk_blocks[bi_ + 1][3])))
                    (Wq, kq), (Ws, ks_) = loaded[bi_]
                    for tb in range(4):
                        p1 = 2 + 2 * (it % 2)
                        p2 = p1 + 1
                        i2 = it % 2
                        it += 1
                        for kk in range(8):
                            op("pe", lambda h: h.matmul(ps[p1][:, :], lhsT=Wq[:, kk, :],
                                                        rhs=UT[:, kk, tb * 512:(tb + 1) * 512],
                                                        start=(kk == 0), stop=(kk == 7)),
                               r=[kq, (utag, kk, tb)], w=["ps%d" % p1])
                        for kk in range(8):
                            op("pe", lambda h: h.matmul(ps[p2][:, :], lhsT=Ws[:, kk, :],
                                                        rhs=UT[:, kk, tb * 512:(tb + 1) * 512],
                                                        start=(kk == 0), stop=(kk == 7)),
                               r=[ks_, (utag, kk, tb)], w=["ps%d" % p2])
                        op("dve", lambda h: h.tensor_tensor(out=ta[i2][:], in0=ps[p1][:, :],
                                                             in1=cosT[:, tb * 512:(tb + 1) * 512], op=ALU.mult),
                           r=["ps%d" % p1, "cosT"], w=["ta%d" % i2])
                        op("dve", lambda h: h.tensor_tensor(out=tb_[i2][:], in0=ps[p2][:, :],
                                                             in1=sinT[:, tb * 512:(tb + 1) * 512], op=ALU.mult),
                           r=["ps%d" % p2, "sinT"], w=["tb%d" % i2])
                        op("pool", lambda h: h.tensor_tensor(out=dst[:, c, tb * 512:(tb + 1) * 512], in0=ta[i2][:],
                                                              in1=tb_[i2][:], op=ALU.add),
                           r=["ta%d" % i2, "tb%d" % i2], w=[(dname, c, tb)])
            for t in range(NT):
                pv = 2 + (t % 2)
                for c in range(4):
                    for kk in range(8):
                        op("pe", lambda h: h.matmul(ps[pv][:, c * 128:(c + 1) * 128],
                                                    lhsT=UT[:, kk, t * 128:(t + 1) * 128], rhs=WV[:, c, kk, :],
                                                    start=(kk == 0), stop=(kk == 7)),
                           r=[("WV", c), (utag, kk, t // 4)], w=["ps%d" % pv])
                op("act", lambda h: h.activation(out=VA[:, t, :, 0:64],
                                                 in_=ps[pv][:, :].rearrange("p (h d) -> p h d", d=64),
                                                 func=AF.Copy), r=["ps%d" % pv, "VA"], w=[("VA", t)])
            if layer == 0:
                k.dbg("QT", QT[:], [128, 4, S], BF16, r=[("QT", c, tb) for c in range(4) for tb in range(4)])
                k.dbg("KT", KT[:], [128, 4, S], BF16, r=[("KT", c, tb) for c in range(4) for tb in range(4)])
            if stop == "qkv":
                k.barrier()
                return True
            items = []
            for qb in range(4):
                for hd in range(8):
                    kts = list(range(max(0, 4 * qb - 8), min(15, 4 * qb + 3 + 8) + 1))
                    for kt in kts:
                        items.append((qb, hd, kt, kt == kts[0], kt == kts[-1]))
            NI = len(items)
            PD = 3

            def emit_score(i):
                qb, hd, kt, _, _ = items[i]
                c = hd // 2
                pb = (hd % 2) * 64
                sbk = i % 4
                op("pe", lambda h: h.matmul(ps[sbk][:, :], lhsT=KT[pb:pb + 64, c, kt * 128:(kt + 1) * 128],
                                            rhs=QT[pb:pb + 64, c, qb * 512:(qb + 1) * 512], start=True, stop=True),
                   r=[("KT", c, kt // 4), ("QT", c, qb)], w=["ps%d" % sbk])

            def emit_rest(i):
                qb, hd, kt, isf, isl = items[i]
                sbk = i % 4
                ei = i % 3
                ob = 6 + (hd % 2)
                okey = "ps%d" % ob
                if isf:
                    op("pe", lambda h: h.matmul(ps[ob][:, 0:260], lhsT=zer[:, 0:128], rhs=zer[:, 0:260],
                                                start=True, stop=False, skip_group_check=True),
                       r=["zer"], w=[okey])
                op("act", lambda h: h.activation(out=Eb[ei][:], in_=ps[sbk][:, :], func=AF.Exp, scale=0.125),
                   r=["ps%d" % sbk], w=["Eb%d" % ei])
                cs = C0 - 128 * (kt - 4 * qb)
                meng = "dve" if (i % 4 != 3) else "pool"
                op(meng, lambda h: h.tensor_tensor(out=Em[ei][:], in0=Eb[ei][:], in1=strip[:, cs:cs + 512],
                                                   op=ALU.mult), r=["Eb%d" % ei], w=["Em%d" % ei])
                for j in range(4):
                    qt = 4 * qb + j
                    if abs(kt - qt) > 8:
                        continue
                    op("pe", lambda h: h.matmul(ps[ob][:, j * 65:(j + 1) * 65],
                                                lhsT=Em[ei][:, j * 128:(j + 1) * 128], rhs=VA[:, kt, hd, :],
                                                start=False, stop=(kt == min(15, qt + 8)), skip_group_check=True),
                       r=["Em%d" % ei, ("VA", kt)], w=[okey])
                if not isl:
                    return
                ov = ps[ob][:, 0:260].rearrange("p (j e) -> p j e", e=65)
                op("dve", lambda h: h.reciprocal(out=rcp[:], in_=ov[:, :, 64]), r=[okey], w=["rcp"])
                op("dve", lambda h: h.tensor_tensor(out=ATT[:, :, hd * 64:(hd + 1) * 64], in0=ov[:, :, 0:64],
                                                     in1=rcp[:].unsqueeze(2).to_broadcast([128, 4, 64]),
                                                     op=ALU.mult), r=[okey, "rcp"], w=[("ATT", hd)])
                if hd != 7:
                    return
                if layer == 0 and qb == 0:
                    k.dbg("ATT0", ATT[:], [128, 4, 512], r=[("ATT", h_) for h_ in range(8)])
                akeys = [("ATT", h_) for h_ in range(8)]
                for j in range(4):
                    op("act", lambda h: h.activation(out=junk[:], in_=ATT[:, j, :], func=AF.Square,
                                                     accum_out=ss[:, j:j + 1]), r=akeys, w=["junk", ("ss", j)])
                op("act", lambda h: h.activation(out=ss[:], in_=ss[:], func=AF.Sqrt, scale=1.0 / 512.0,
                                                 bias=C["eps_rms"][:, 0:1]),
                   r=[("ss", j) for j in range(4)], w=["ssq"])
                op("dve", lambda h: h.reciprocal(out=ss[:], in_=ss[:]), r=["ssq"], w=["ssr"])
                for j in range(4):
                    t = 4 * qb + j
                    a2 = j % 2
                    op("dve", lambda h: h.scalar_tensor_tensor(out=ATB[a2][:], in0=ATT[:, j, :], scalar=ss[:, j:j + 1],
                                                                in1=AG[:], op0=ALU.mult, op1=ALU.mult),
                       r=akeys + ["ssr", "AG"], w=["ATB%d" % a2])
                    pt = 4 + (j % 2)
                    pbf = ps[pt][:, :].bitcast(BF16)
                    for cc in range(4):
                        op("pe", lambda h: h.transpose(out=pbf[:, cc * 128:(cc + 1) * 128],
                                                       in_=ATB[a2][:, cc * 128:(cc + 1) * 128], identity=ident_b[:]),
                           r=["ATB%d" % a2, "ident_b"], w=["ps%d" % pt])
                    op("act", lambda h: h.activation(out=CATT[:, 0:4, t * 128:(t + 1) * 128],
                                                     in_=pbf[:, 0:512].rearrange("p (c t) -> p c t", t=128),
                                                     func=AF.Copy), r=["ps%d" % pt], w=[("CATT", 0, t)])

            for i in range(NI + PD):
                if i < NI:
                    emit_score(i)
                if i >= PD:
                    emit_rest(i - PD)
            k.barrier()
        if stop == "attn":
            return True
        with ExitStack() as rs:
            lb, oml, mu, ml = C["lb"], C["oml"], C["mui"], C["mli"]
            RI = sb(rs, "RI", [128, NT, 512], BF16)
            WR4 = sb(rs, "WR4", [128, 4, 8, 128], BF16)
            wp = WPool(k, rs, wb, 4, T + "r")
            RQ = sb(rs, "RQ", [128, S], BF16)
            FB = sb(rs, "FB", [128, S], F32)
            KK = sb(rs, "KK", [128, S], BF16)
            GX = sb(rs, "GX", [128, S + 1], F32)
            E1 = sb(rs, "E1", [128, S], BF16)
            E2 = sb(rs, "E2", [128, S], BF16)
            QD = sb(rs, "QD", [128, S], BF16)
            KD = sb(rs, "KD", [128, S], BF16)
            KTOK = sb(rs, "KTOK", [128, NT, 128], BF16)
            SC = sb(rs, "SC", [128, 3, 32], F32)
            AMA = [sb(rs, "AMA", [128, 2, NT, 64], BF16) for _ in range(2)]
            Sst = sb(rs, "Sst", [128, 128], F32)
            DSg = sb(rs, "DSg", [128, 32, 128], BF16)
            SAA = sb(rs, "SAA", [128, 32, 128], BF16)
            RECO = sb(rs, "RECO", [128, NT, 128], F32)
            RECN = sb(rs, "RECN", [128, NT, 512], BF16)
            rst = sb(rs, "rst", [128, NT], F32)
            RGN = sb(rs, "RGN", [128, 512], F32)
            SG = [sb(rs, "SG", [128, 512], F32) for _ in range(1)]
            RB = [sb(rs, "RB", [128, 512], BF16) for _ in range(1)]
            dma("sp", RGN[:], C["rgain_in"][layer:layer + 1, :].to_broadcast([128, 512]), w=["RGN"])
            zst = sb(rs, "zst", [128, 128], BF16)
            op("dve", lambda h: h.memset(zst[:], 0.0), w=["zst"])
            for d in range(2):
                op("dve", lambda h: h.memset(AMA[d][:], 0.0), w=[("AMA", d, 0), ("AMA", d, 1)])
            for c in range(4):
                dma("pool", WR4[:, c, :, :], wb[32 + c].rearrange("p (k c) -> p k c", c=128), w=[("WR4", c)])
            for t in range(NT):
                pv = t % 2
                for c in range(4):
                    for kk in range(8):
                        op("pe", lambda h: h.matmul(ps[pv][:, c * 128:(c + 1) * 128],
                                                    lhsT=UT[:, kk, t * 128:(t + 1) * 128], rhs=WR4[:, c, kk, :],
                                                    start=(kk == 0), stop=(kk == 7)),
                           r=[("WR4", c), (utag, kk, t // 4)], w=["ps%d" % pv])
                op("act", lambda h: h.activation(out=RI[:, t, :], in_=ps[pv][:, :], func=AF.Copy),
                   r=["ps%d" % pv], w=[("RI", t)])
            for c in range(4):
                dma("pool", WR4[:, c, :, :], wb[36 + c].rearrange("p (k c) -> p k c", c=128), w=[("WR4", c)])
            op("dve", lambda h: h.memset(GX[:, 0:1], 0.0), w=["GX"])
            nmm = 0
            for hh in range(4):
                Wq, kq = wp.load(20 + hh)
                for tb in range(4):
                    pb_ = nmm % 2
                    nmm += 1
                    for kk in range(8):
                        op("pe", lambda h: h.matmul(ps[pb_][:, :], lhsT=Wq[:, kk, :],
                                                    rhs=UT[:, kk, tb * 512:(tb + 1) * 512],
                                                    start=(kk == 0), stop=(kk == 7)),
                           r=[kq, (utag, kk, tb)], w=["ps%d" % pb_])
                    op("act", lambda h: h.activation(out=RQ[:, tb * 512:(tb + 1) * 512], in_=ps[pb_][:, :],
                                                     func=AF.Silu), r=["ps%d" % pb_], w=["RQ"])
                for d in range(2):
                    Wf, kf_ = wp.load(24 + 4 * d + hh)
                    col = layer * 8 + d * 4 + hh
                    for tb in range(4):
                        pb_ = nmm % 2
                        nmm += 1
                        for kk in range(8):
                            op("pe", lambda h: h.matmul(ps[pb_][:, :], lhsT=Wf[:, kk, :],
                                                        rhs=UT[:, kk, tb * 512:(tb + 1) * 512],
                                                        start=(kk == 0), stop=(kk == 7)),
                               r=[kf_, (utag, kk, tb)], w=["ps%d" % pb_])
                        op("act", lambda h: h.activation(out=FB[:, tb * 512:(tb + 1) * 512], in_=ps[pb_][:, :],
                                                         func=AF.Sigmoid), r=["ps%d" % pb_], w=["FB"])
                    op("dve", lambda h: h.tensor_scalar(out=FB[:], in0=FB[:], scalar1=oml[:, col:col + 1],
                                                         scalar2=lb[:, col:col + 1], op0=ALU.mult, op1=ALU.add),
                       r=["FB", "lb", "oml"], w=["FB"])
                    op("dve", lambda h: h.tensor_scalar(out=KK[:], in0=FB[:], scalar1=-1.0, scalar2=1.0,
                                                         op0=ALU.mult, op1=ALU.add), r=["FB"], w=["KK"])
                    op("act", lambda h: h.activation(out=FB[:], in_=FB[:], func=AF.Ln), r=["FB", "KK"], w=["FB"])
                    op("dve", lambda h: h.tensor_tensor_scan(out=GX[:, 1:S + 1],
                                                              data0=ones_f[:, 0:1].to_broadcast([128, S]),
                                                              data1=FB[:], initial=0.0, op0=ALU.mult, op1=ALU.add),
                       r=["FB", "ones_f"], w=["GX"])
                    if layer == 0 and hh == 0:
                        k.dbg("GX%d" % d, GX[:], [128, S + 1], r=["GX"])
                    s0 = GX[:, 0:S:64]
                    sm = GX[:, 32:S + 1:64]
                    s1 = GX[:, 64:S + 1:64]
                    tok = GX[:, 1:S + 1] if d == 0 else GX[:, 0:S]
                    op("dve", lambda h: h.tensor_tensor(out=FB[:].rearrange("p (n t) -> p n t", t=64),
                                                         in0=tok.rearrange("p (n t) -> p n t", t=64),
                                                         in1=sm.unsqueeze(2).to_broadcast([128, 32, 64]),
                                                         op=ALU.subtract), r=["GX", "FB"], w=["FB"])
                    b1 = C["nsh"][:, 0:1] if d == 1 else C["zero1"][:, 0:1]
                    b2 = C["nsh"][:, 0:1] if d == 0 else C["zero1"][:, 0:1]
                    op("act", lambda h: h.activation(out=E1[:], in_=FB[:], func=AF.Exp, bias=b1), r=["FB"], w=["E1"])
                    op("act", lambda h: h.activation(out=E2[:], in_=FB[:], func=AF.Exp, scale=-1.0, bias=b2),
                       r=["FB"], w=["E2"])
                    eq, ek = (E1, E2) if d == 0 else (E2, E1)
                    op("dve", lambda h: h.tensor_tensor(out=QD[:], in0=RQ[:], in1=eq[:], op=ALU.mult),
                       r=["RQ", "E1", "E2"], w=["QD"])
                    op("dve", lambda h: h.tensor_tensor(out=KD[:], in0=KK[:], in1=ek[:], op=ALU.mult),
                       r=["KK", "E1", "E2"], w=["KD"])
                    for idx, (a_, b_) in enumerate(((sm, s0), (s1, s0), (s1, sm))):
                        op("dve", lambda h: h.tensor_tensor(out=SC[:, idx, :], in0=a_, in1=b_, op=ALU.subtract),
                           r=["GX", "SCe"], w=[("SC", idx)])
                    op("act", lambda h: h.activation(out=SC[:], in_=SC[:], func=AF.Exp),
                       r=[("SC", i_) for i_ in range(3)], w=["SCe"] + [("SC", i_) for i_ in range(3)])
                    for t4 in range(4):
                        pbf = ps[pb_ := (nmm % 2)][:, :].bitcast(BF16)
                        nmm += 1
                        for tt in range(4):
                            t = 4 * t4 + tt
                            op("pe", lambda h: h.transpose(out=pbf[:, tt * 128:(tt + 1) * 128],
                                                           in_=KD[:, t * 128:(t + 1) * 128], identity=ident_b[:]),
                               r=["KD", "ident_b"], w=["ps%d" % pb_])
                        op("act", lambda h: h.activation(out=KTOK[:, 4 * t4:4 * t4 + 4, :],
                                                         in_=pbf[:, 0:512].rearrange("p (c t) -> p c t", t=128),
                                                         func=AF.Copy), r=["ps%d" % pb_], w=["KTOK"])
                    ia, ig = (0, 2) if d == 0 else (2, 0)
                    msk = mu if d == 0 else ml
                    hc = slice(hh * 128, (hh + 1) * 128)
                    for bb in range(2):
                        bank = 2 + bb
                        for t8 in range(8):
                            t = bb * 8 + t8
                            for hf in range(2):
                                n = 2 * t + hf
                                po = hf * 64
                                tk = slice(n * 64, n * 64 + 64)
                                op("pe", lambda h: h.matmul(ps[bank][po:po + 64, t8 * 64:(t8 + 1) * 64],
                                                            lhsT=KD[:, tk], rhs=QD[:, tk], start=True, stop=True),
                                   r=["KD", "QD"], w=["ps%d" % bank])
                        for hf in range(2):
                            hs = slice(hf * 64, (hf + 1) * 64)
                            op("dve", lambda h: h.copy_predicated(
                                out=AMA[d][hs, hf, bb * 8:(bb + 1) * 8, :],
                                mask=msk[hs, :, :],
                                data=ps[bank][hs, :].rearrange("p (t s) -> p t s", s=64)),
                               r=["ps%d" % bank, "mui", "mli"], w=[("AMA", d, bb)])
                    for Q in range(4):
                        bk0 = 4 if Q % 2 == 0 else 2
                        for c8 in range(8):
                            n = 8 * Q + c8
                            t = n // 2
                            po = (n % 2) * 64
                            bank = bk0 + (n % 2)
                            cc = c8 // 2
                            op("pe", lambda h: h.matmul(ps[bank][:, cc * 128:(cc + 1) * 128],
                                                        lhsT=KTOK[po:po + 64, t, :], rhs=RI[po:po + 64, t, hc],
                                                        start=True, stop=True),
                               r=["KTOK", ("RI", t)], w=["ps%d" % bank])
                        for par in range(2):
                            bank = bk0 + par
                            lo = 8 * Q + par
                            op("dve", lambda h: h.tensor_tensor(
                                out=DSg[:, lo:lo + 7:2, :], in0=ps[bank][:, :].rearrange("p (c v) -> p c v", v=128),
                                in1=SC[:, ig, lo:lo + 7:2].unsqueeze(2).to_broadcast([128, 4, 128]), op=ALU.mult),
                               r=["ps%d" % bank, "SCe"], w=[("DSg", Q)])
                    order = list(range(32)) if d == 0 else list(range(31, -1, -1))
                    prev = {order[i + 1]: order[i] for i in range(31)}
                    op("dve", lambda h: h.tensor_tensor(out=QD[:].rearrange("p (n t) -> p n t", t=64),
                                                         in0=QD[:].rearrange("p (n t) -> p n t", t=64),
                                                         in1=SC[:, ia, :].unsqueeze(2).to_broadcast([128, 32, 64]),
                                                         op=ALU.mult), r=["QD", "SCe"], w=["QD"])
                    for i, n in enumerate(order[:-1]):
                        if i == 0:
                            op("dve", lambda h: h.tensor_copy(out=SAA[:, n, :], in_=DSg[:, n, :]),
                               r=[("DSg", n // 8)], w=[("SAA", n)])
                        else:
                            p_ = prev[n]
                            op("dve", lambda h: h.scalar_tensor_tensor(out=SAA[:, n, :], in0=SAA[:, p_, :],
                                                                        scalar=SC[:, 1, n:n + 1], in1=DSg[:, n, :],
                                                                        op0=ALU.mult, op1=ALU.add),
                               r=[("SAA", p_), ("DSg", n // 8), "SCe"], w=[("SAA", n)])
                    qorder = list(range(4)) if d == 0 else list(range(3, -1, -1))
                    for q in qorder:
                        bank = 6 + (q % 2)
                        for t4 in range(4):
                            t = 4 * q + t4
                            for hf in range(2):
                                n = 2 * t + hf
                                po = hf * 64
                                tk = slice(n * 64, n * 64 + 64)
                                firstn = (n == order[0])
                                op("pe", lambda h: h.matmul(ps[bank][po:po + 64, t4 * 128:(t4 + 1) * 128],
                                                            lhsT=AMA[d][:, hf, t, :], rhs=RI[:, t, hc],
                                                            start=True, stop=False),
                                   r=[("AMA", d, t // 8), ("RI", t)], w=["ps%d" % bank])
                                if n == order[0]:
                                    st_ap, st_key = zst[:, :], "zst"
                                else:
                                    st_ap, st_key = SAA[:, prev[n], :], ("SAA", prev[n])
                                op("pe", lambda h: h.matmul(ps[bank][po:po + 64, t4 * 128:(t4 + 1) * 128],
                                                            lhsT=QD[:, tk], rhs=st_ap,
                                                            start=False, stop=True),
                                   r=["QD", st_key], w=["ps%d" % bank])
                        pv_ = ps[bank][:, :].rearrange("p (t v) -> p t v", v=128)
                        if d == 0:
                            op("act", lambda h: h.activation(out=RECO[:, 4 * q:4 * q + 4, :], in_=pv_, func=AF.Copy,
                                                             scale=math.exp(KSH)),
                               r=["ps%d" % bank], w=[("RECO", q)])
                        else:
                            op("dve", lambda h: h.scalar_tensor_tensor(out=RECO[:, 4 * q:4 * q + 4, :], in0=pv_,
                                                                        scalar=math.exp(KSH),
                                                                        in1=RECO[:, 4 * q:4 * q + 4, :],
                                                                        op0=ALU.mult, op1=ALU.add),
                               r=["ps%d" % bank, ("RECO", q)], w=[("RECO", q)])
                rkeys = [("RECO", q) for q in range(4)]
                if layer == 0 and hh == 0:
                    k.dbg("RECO0", RECO[:], [128, NT, 128], r=rkeys)
                fb3 = FB[:].rearrange("p (t v) -> p t v", v=128)
                op("dve", lambda h: h.tensor_tensor(out=fb3, in0=RECO[:], in1=RECO[:], op=ALU.mult),
                   r=rkeys + ["FB"], w=["FB"])
                op("dve", lambda h: h.tensor_reduce(out=rst[:], in_=fb3, axis=AX.X, op=ALU.add), r=["FB"], w=["rst"])
                op("act", lambda h: h.activation(out=rst[:], in_=rst[:], func=AF.Sqrt, scale=1.0 / 128.0,
                                                 bias=C["eps_rms"][:, 0:1]), r=["rst"], w=["rst"])
                op("dve", lambda h: h.reciprocal(out=rst[:], in_=rst[:]), r=["rst"], w=["rst"])
                op("dve", lambda h: h.tensor_tensor(out=fb3, in0=RECO[:],
                                                     in1=rst[:].unsqueeze(2).to_broadcast([128, NT, 128]),
                                                     op=ALU.mult), r=rkeys + ["rst", "FB"], w=["FB"])
                op("dve", lambda h: h.tensor_tensor(out=RECN[:, :, hh * 128:(hh + 1) * 128], in0=fb3,
                                                     in1=RGN[:, hh * 128:(hh + 1) * 128].unsqueeze(1)
                                                     .to_broadcast([128, NT, 128]), op=ALU.mult),
                   r=["FB", "RGN"] + rkeys, w=[("RECN", hh)])
            for t in range(NT):
                pv = t % 2
                i2 = 0
                for c in range(4):
                    for kk in range(8):
                        op("pe", lambda h: h.matmul(ps[pv][:, c * 128:(c + 1) * 128],
                                                    lhsT=UT[:, kk, t * 128:(t + 1) * 128], rhs=WR4[:, c, kk, :],
                                                    start=(kk == 0), stop=(kk == 7)),
                           r=[("WR4", c), (utag, kk, t // 4)], w=["ps%d" % pv])
                op("act", lambda h: h.activation(out=SG[i2][:], in_=ps[pv][:, :], func=AF.Sigmoid),
                   r=["ps%d" % pv], w=["SG%d" % i2])
                op("dve", lambda h: h.tensor_tensor(out=RB[i2][:], in0=RECN[:, t, :], in1=SG[i2][:], op=ALU.mult),
                   r=["SG%d" % i2] + [("RECN", hh) for hh in range(4)], w=["RB%d" % i2])
                pt = 2 + (t % 2)
                pbf = ps[pt][:, :].bitcast(BF16)
                for cc in range(4):
                    op("pe", lambda h: h.transpose(out=pbf[:, cc * 128:(cc + 1) * 128],
                                                   in_=RB[i2][:, cc * 128:(cc + 1) * 128], identity=ident_b[:]),
                       r=["RB%d" % i2, "ident_b"], w=["ps%d" % pt])
                op("act", lambda h: h.activation(out=CATT[:, 4:8, t * 128:(t + 1) * 128],
                                                 in_=pbf[:, 0:512].rearrange("p (c t) -> p c t", t=128),
                                                 func=AF.Copy), r=["ps%d" % pt], w=[("CATT", 1, t)])
            k.barrier()
        if stop == "rec":
            return True
        with ExitStack() as os_:
            WO = sb(os_, "WO", [128, 8, D], BF16)
            for kk in range(8):
                dma("pool", WO[:, kk, :], C["wo_in"][layer, kk * 128:(kk + 1) * 128, :], w=[("WO", kk)])
            wkeys = [("WO", kk) for kk in range(8)]

            def ymm(t, half, bank, bkey):
                for kk in range(8):
                    op("pe", lambda h: h.matmul(ps[bank][:, :], lhsT=CATT[:, kk, t * 128:(t + 1) * 128],
                                                rhs=WO[:, kk, half * 512:(half + 1) * 512],
                                                start=(kk == 0), stop=(kk == 7)), r=wkeys, w=[bkey])
            resid_ln(k, C, os_, ls, ymm, x_src=x_src, x_dst=x_dst, tag=T + "o")
            k.barrier()


def resid_ln(k, C, es, ls, ymm, x_src, x_dst, tag, X=None):
    op, dma, ps, sb = k.op, k.dma, k.ps, k.sb
    if X is None:
        grow = sb(es, "grow", [128, D], F32)
        dma("sp", grow[:], C["grow_d"][ls], w=["grow"])
    LNG = sb(es, "LNG", [128, D], F32)
    LNB = sb(es, "LNB", [128, D], F32)
    dma("sp", LNG[:], C["lng_in"][ls:ls + 1, :].to_broadcast([128, D]), w=[tag + "LNG"])
    dma("sp", LNB[:], C["lnb_in"][ls:ls + 1, :].to_broadcast([128, D]), w=[tag + "LNB"])
    NB = 3
    xt = [sb(es, "xt", [128, D], F32) for _ in range(NB)]
    zt = [sb(es, "zt", [128, D], F32) for _ in range(NB)] if X is None else None
    st = [sb(es, "st", [128, 2, 6], F32) for _ in range(NB)]
    mv = [sb(es, "mv", [128, 2], F32) for _ in range(NB)]
    rs_ = [sb(es, "rs", [128, 2], F32) for _ in range(NB)]
    for t in range(NT):
        i2 = t % NB
        zk = tag + "zt%d" % i2
        if X is None:
            xk = tag + "xt%d" % i2
            dma("sp", xt[i2][:], x_src[t * 128:(t + 1) * 128, :], w=[xk])
            for half in range(2):
                bank = 2 + 2 * i2 + half
                bkey = "ps%d" % bank
                ymm(t, half, bank, bkey)
                op("dve", lambda h: h.tensor_tensor(out=zt[i2][:, half * 512:(half + 1) * 512], in0=ps[bank][:, :],
                                                     in1=grow[:, half * 512:(half + 1) * 512], op=ALU.mult),
                   r=[bkey, "grow"], w=[zk])
            op("dve", lambda h: h.scalar_tensor_tensor(out=zt[i2][:], in0=xt[i2][:], scalar=ALPHA, in1=zt[i2][:],
                                                        op0=ALU.mult, op1=ALU.add), r=[xk, zk], w=[zk])
            z = zt[i2][:, :]
            zkeys = [zk]
        else:
            z = X[:, t, :]
            zkeys = [("X", t, 0), ("X", t, 1)]
        for half in range(2):
            op("dve", lambda h: h.bn_stats(out=st[i2][:, half, :], in_=z[:, half * 512:(half + 1) * 512]),
               r=zkeys, w=[tag + "st%d" % i2])
        op("dve", lambda h: h.bn_aggr(out=mv[i2][:], in_=st[i2][:].rearrange("p a b -> p (a b)")),
           r=[tag + "st%d" % i2], w=[tag + "mv%d" % i2])
        op("act", lambda h: h.activation(out=rs_[i2][:, 0:1], in_=mv[i2][:, 1:2], func=AF.Sqrt,
                                         bias=C["eps_ln"][:, 0:1]), r=[tag + "mv%d" % i2], w=[tag + "rs%d" % i2])
        op("dve", lambda h: h.reciprocal(out=rs_[i2][:, 0:1], in_=rs_[i2][:, 0:1]),
           r=[tag + "rs%d" % i2], w=[tag + "rs%d" % i2])
        op("dve", lambda h: h.scalar_tensor_tensor(out=rs_[i2][:, 1:2], in0=mv[i2][:, 0:1], scalar=-1.0,
                                                    in1=rs_[i2][:, 0:1], op0=ALU.mult, op1=ALU.mult),
           r=[tag + "mv%d" % i2, tag + "rs%d" % i2], w=[tag + "rs%d" % i2])
        ok = tag + "on%d" % i2
        o = xt[i2]
        op("act", lambda h: h.activation(out=o[:], in_=z, func=AF.Identity, scale=rs_[i2][:, 0:1],
                                         bias=rs_[i2][:, 1:2]),
           r=zkeys + [tag + "rs%d" % i2, tag + "xt%d" % i2], w=[ok, tag + "xt%d" % i2])
        op("dve", lambda h: h.tensor_tensor(out=o[:], in0=o[:], in1=LNG[:], op=ALU.mult),
           r=[ok, tag + "LNG"], w=[ok, tag + "xt%d" % i2])
        op("pool", lambda h: h.tensor_tensor(out=o[:], in0=o[:], in1=LNB[:], op=ALU.add),
           r=[ok, tag + "LNB"], w=[ok, tag + "xt%d" % i2])
        ev = dma("pool", x_dst[t * 128:(t + 1) * 128, :], o[:], r=[ok, tag + "xt%d" % i2], w=[(tag + "dst", t)])
        if C.get("_final"):
            k.out_events.append(ev)


def ffn(k, C, layer, x_src, x_dst, is_final):
    nc, op, dma, ps, sb = k.nc, k.op, k.dma, k.ps, k.sb
    ls = layer * 2 + 1
    T = "f%d" % layer
    utag = T + "UT"
    moe = (layer % 2 == 1)
    C["_final"] = is_final
    with ExitStack() as fs:
        X = sb(fs, "X", [128, NT, D], F32)
        UT = sb(fs, "UT", [128, 8, S], BF16)
        HT = sb(fs, "HT", [128, 4, S], BF16)
        WG = [sb(fs, "WG", [128, 8, 512], BF16) for _ in range(2)]
        WU = [sb(fs, "WU", [128, 8, 512], BF16) for _ in range(2)]
        WD = [sb(fs, "WD", [128, 4, D], BF16) for _ in range(2)]
        SGb = [sb(fs, "SGb", [128, 512], F32) for _ in range(2)]
        for g in range(4):
            dma("sp", X[:, 4 * g:4 * g + 4, :], x_src[g * 512:(g + 1) * 512, :].rearrange("(t p) d -> p t d", p=128),
                w=[("X", 4 * g + t, hf) for t in range(4) for hf in range(2)])
        router = None
        if moe:
            WR = sb(fs, "WR", [128, 8, NE], F32)
            LG = sb(fs, "LG", [128, NT, NE], F32)
            L2 = sb(fs, "L2", [128, NT, NE], F32)
            EQ1 = sb(fs, "EQ1", [128, NT, NE], F32)
            EQ2 = sb(fs, "EQ2", [128, NT, NE], F32)
            COMB = sb(fs, "COMB", [128, NT, NE], F32)
            m1 = sb(fs, "m1", [128, NT], F32)
            m2 = sb(fs, "m2", [128, NT], F32)
            w1 = sb(fs, "w1", [128, NT], F32)
            w2 = sb(fs, "w2", [128, NT], F32)
            us = ExitStack()
            UF = sb(us, "UF", [128, 8, 512], F32)
            with nc.allow_non_contiguous_dma(reason="tiny router weight"):
                dma("sp", WR[:], C["mr_in"].rearrange("(k p) e -> p k e", p=128), w=["WR"])

            def router(g, kk, pb, pk):
                op("act", lambda h: h.activation(out=UF[:, kk, :], in_=pb[:, :], func=AF.Identity,
                                                 scale=C["adac"][:, ls, 8 + kk:9 + kk],
                                                 bias=C["adac"][:, ls, kk:kk + 1]),
                   r=[pk, "adac"], w=[("UF", kk)])
                if kk == 7:
                    for t in range(4):
                        for k2 in range(8):
                            op("pe", lambda h: h.matmul(ps[2][:, t * 8:(t + 1) * 8],
                                                        lhsT=UF[:, k2, t * 128:(t + 1) * 128], rhs=WR[:, k2, :],
                                                        start=(k2 == 0), stop=(k2 == 7)),
                               r=[("UF", k2), "WR"], w=["ps2"])
                    op("dve", lambda h: h.tensor_copy(out=LG[:, 4 * g:4 * g + 4, :],
                                                      in_=ps[2][:, 0:32].rearrange("p (t e) -> p t e", e=NE)),
                       r=["ps2"], w=[("LG", g)])
        build_ut(k, C, UT, utag, ls, X=X, router=router)
        if moe:
            k.barrier()
            us.close()
        for t in range(NT):
            op("dve", lambda h: h.tensor_scalar(out=X[:, t, :], in0=X[:, t, :], scalar1=ALPHA, scalar2=None,
                                                op0=ALU.mult), r=[("X", t, 0), ("X", t, 1)], w=[("X", t, 0), ("X", t, 1)])
        grow = sb(fs, "growf", [128, D], F32)
        growb = sb(fs, "growb", [128, D], BF16)
        dma("sp", grow[:], C["grow_d"][ls], w=["growf"])
        op("dve", lambda h: h.tensor_copy(out=growb[:], in_=grow[:]), r=["growf"], w=["growb"])
        if moe:
            lgk = [("LG", g) for g in range(4)]
            bc = lambda a: a[:].unsqueeze(2).to_broadcast([128, NT, NE])
            op("dve", lambda h: h.tensor_reduce(out=m1[:], in_=LG[:], axis=AX.X, op=ALU.max), r=lgk, w=["m1"])
            op("dve", lambda h: h.tensor_tensor(out=EQ1[:], in0=LG[:], in1=bc(m1), op=ALU.is_equal),
               r=lgk + ["m1"], w=["EQ1"])
            op("dve", lambda h: h.scalar_tensor_tensor(out=L2[:], in0=EQ1[:], scalar=-1e30, in1=LG[:],
                                                        op0=ALU.mult, op1=ALU.add), r=lgk + ["EQ1"], w=["L2"])
            op("dve", lambda h: h.tensor_reduce(out=m2[:], in_=L2[:], axis=AX.X, op=ALU.max), r=["L2"], w=["m2"])
            op("dve", lambda h: h.tensor_tensor(out=EQ2[:], in0=L2[:], in1=bc(m2), op=ALU.is_equal),
               r=["L2", "m2"], w=["EQ2"])
            op("dve", lambda h: h.tensor_tensor(out=w2[:], in0=m1[:], in1=m2[:], op=ALU.subtract),
               r=["m1", "m2"], w=["w2"])
            op("act", lambda h: h.activation(out=w1[:], in_=w2[:], func=AF.Sigmoid), r=["w2"], w=["w1"])
            op("dve", lambda h: h.tensor_scalar(out=w2[:], in0=w1[:], scalar1=-1.0, scalar2=1.0, op0=ALU.mult,
                                                 op1=ALU.add), r=["w1", "w2"], w=["w2"])
            op("dve", lambda h: h.tensor_tensor(out=COMB[:], in0=EQ1[:], in1=bc(w1), op=ALU.mult),
               r=["EQ1", "w1"], w=["COMB"])
            op("dve", lambda h: h.tensor_tensor(out=EQ2[:], in0=EQ2[:], in1=bc(w2), op=ALU.mult),
               r=["EQ2", "w2"], w=["EQ2"])
            op("dve", lambda h: h.tensor_tensor(out=COMB[:], in0=COMB[:], in1=EQ2[:], op=ALU.add),
               r=["COMB", "EQ2"], w=["COMB"])
            k.dbg("COMB", COMB[:], [128, NT, NE], r=["COMB"])
        gi = 0
        it = 0
        ie = 0
        for e in (range(NE) if moe else [None]):
            dff = DFE if moe else DFF
            wg_src = C["mwg_in"][e] if moe else C["fwg_in"]
            wu_src = C["mwu_in"][e] if moe else C["fwu_in"]
            wd_src = C["mwd_in"][e] if moe else C["fwd_in"]
            wgv = wg_src.rearrange("(k p) f -> p k f", p=128)
            wuv = wu_src.rearrange("(k p) f -> p k f", p=128)
            for g in range((dff + 511) // 512):
                f0 = g * 512
                fw = min(512, dff - f0)
                chunks = [(c0, min(128, fw - c0)) for c0 in range(0, fw, 128)]
                b = gi % 2
                gi += 1
                dma("pool", WG[b][:, :, 0:fw], wgv[:, :, f0:f0 + fw], w=[("WG", b)])
                dma("pool", WU[b][:, :, 0:fw], wuv[:, :, f0:f0 + fw], w=[("WU", b)])
                for j, (c0, cs) in enumerate(chunks):
                    dma("pool", WD[b][0:cs, j, :], wd_src[f0 + c0:f0 + c0 + cs, :], w=[("WD", b, j)])
                for j, (c0, cs) in enumerate(chunks):
                    op("pool", lambda h: h.tensor_tensor(out=WD[b][0:cs, j, :], in0=WD[b][0:cs, j, :],
                                                         in1=growb[0:cs, :], op=ALU.mult),
                       r=["growb"], w=[("WD", b, j)])
                for j, (c0, cs) in enumerate(chunks):
                    for tb in range(4):
                        pg = (it % 2) * 2
                        pu = pg + 1
                        i2 = it % 2
                        it += 1
                        for kk in range(8):
                            op("pe", lambda h: h.matmul(ps[pg][0:cs, :], lhsT=WG[b][:, kk, c0:c0 + cs],
                                                        rhs=UT[:, kk, tb * 512:(tb + 1) * 512],
                                                        start=(kk == 0), stop=(kk == 7)),
                               r=[("WG", b), (utag, kk, tb)], w=["ps%d" % pg])
                        for kk in range(8):
                            op("pe", lambda h: h.matmul(ps[pu][0:cs, :], lhsT=WU[b][:, kk, c0:c0 + cs],
                                                        rhs=UT[:, kk, tb * 512:(tb + 1) * 512],
                                                        start=(kk == 0), stop=(kk == 7)),
                               r=[("WU", b), (utag, kk, tb)], w=["ps%d" % pu])
                        op("act", lambda h: h.activation(out=SGb[i2][0:cs, :], in_=ps[pg][0:cs, :], func=AF.Silu),
                           r=["ps%d" % pg], w=["SGb%d" % i2])
                        op("dve", lambda h: h.tensor_tensor(out=HT[0:cs, j, tb * 512:(tb + 1) * 512],
                                                             in0=ps[pu][0:cs, :], in1=SGb[i2][0:cs, :], op=ALU.mult),
                           r=["ps%d" % pu, "SGb%d" % i2], w=[("HT", j, tb)])
                for t in range(NT):
                    for half in range(2):
                        bank = 4 + (ie % 4)
                        i2 = ie % 2
                        ie += 1
                        bkey = "ps%d" % bank
                        for j, (c0, cs) in enumerate(chunks):
                            op("pe", lambda h: h.matmul(ps[bank][:, :], lhsT=HT[0:cs, j, t * 128:(t + 1) * 128],
                                                        rhs=WD[b][0:cs, j, half * 512:(half + 1) * 512],
                                                        start=(j == 0), stop=(j == len(chunks) - 1)),
                               r=[("HT", j, t // 4), ("WD", b, j)], w=[bkey])
                        sc = COMB[:, t, e:e + 1] if moe else 1.0
                        op("dve", lambda h: h.scalar_tensor_tensor(out=X[:, t, half * 512:(half + 1) * 512],
                                                                    in0=ps[bank][:, :], scalar=sc,
                                                                    in1=X[:, t, half * 512:(half + 1) * 512],
                                                                    op0=ALU.mult, op1=ALU.add),
                           r=[bkey, ("X", t, half)] + (["COMB"] if moe else []), w=[("X", t, half)])
        if layer == 0:
            pass
        resid_ln(k, C, fs, ls, None, x_src=None, x_dst=x_dst, tag=T + "o", X=X)
        k.barrier()
```

```python
import math
from contextlib import ExitStack
import numpy as np
import concourse.bass as bass
import concourse.mybir as mybir
from concourse.bass_utils import run_bass_kernel_spmd

F32 = mybir.dt.float32
BF16 = mybir.dt.bfloat16
I32 = mybir.dt.int32
AF = mybir.ActivationFunctionType
ALU = mybir.AluOpType
AX = mybir.AxisListType

S = 2048
D = 1024
NT = 16
DFF = 2752
DFE = 3584
NE = 8
ALPHA = 4.0 ** 0.25
LN_EPS = 1e-5
RMS_EPS = 1e-6
C0 = 1408
STRW = 2944
TWO_PI = 2.0 * math.pi
CW1 = 6.28125
CW2 = TWO_PI - 6.28125
PI_SAFE = 3.1415925
KSH = 30.0


class Eng:
    def __init__(self, name, h, sem):
        self.name = name
        self.h = h
        self.sem = sem
        self.count = 0
        self.waited = {}

    def wait(self, ev):
        sem, val, _ = ev
        key = id(sem)
        if self.waited.get(key, 0) >= val:
            return
        self.h.wait_ge(sem, val)
        self.waited[key] = val


class KB:
    def __init__(self):
        self.nc = bass.Bass("TRN2", target_bir_lowering=False)
        self.es = ExitStack()
        nc = self.nc
        self.E = {}
        for name, h in [("pe", nc.tensor), ("act", nc.scalar), ("dve", nc.vector),
                        ("pool", nc.gpsimd), ("sp", nc.sync)]:
            sem = self.es.enter_context(nc.semaphore("s_" + name))
            self.E[name] = Eng(name, h, sem)
        self.trk = {}
        self.slots = {}
        for q, n in (("sp", 12), ("pool", 12), ("act", 4)):
            self.slots[q] = [[self.es.enter_context(nc.semaphore("d_%s%d" % (q, i))), 0] for i in range(n)]
        self.slot_rr = {"sp": 0, "pool": 0, "act": 0}
        self.out_events = []
        self.ps = [self.es.enter_context(nc.psum_tensor("ps%d" % i, [128, 512], F32)) for i in range(8)]
        self.dbg_outs = {}

    def _deps(self, r, w, engname):
        deps = []
        for k in r:
            t = self.trk.get(k)
            if t and t[0] is not None:
                deps.append(t[0])
        for k in w:
            t = self.trk.get(k)
            if t:
                if t[0] is not None:
                    deps.append(t[0])
                for en, ev in t[1].items():
                    if en != engname:
                        deps.append(ev)
        return deps

    def _update(self, r, w, ev, engname):
        for k in r:
            t = self.trk.setdefault(k, [None, {}])
            t[1][engname] = ev
        for k in w:
            self.trk[k] = [ev, {}]

    def op(self, eng, fn, r=(), w=()):
        e = self.E[eng]
        for d in self._deps(r, w, eng):
            if eng == "pe" and d[2] == "pe":
                continue
            e.wait(d)
        ins = fn(e.h)
        e.count += 1
        ins.then_inc(e.sem, 1)
        ev = (e.sem, e.count, eng)
        self._update(r, w, ev, eng)
        return ev

    def dma(self, q, out, in_, r=(), w=(), **kw):
        e = self.E[q]
        slots = self.slots[q]
        i = self.slot_rr[q]
        self.slot_rr[q] = (i + 1) % len(slots)
        slot = slots[i]
        name = "dma_%s%d" % (q, i)
        for d in self._deps(r, w, name):
            e.wait(d)
        if slot[1] > 0:
            e.wait((slot[0], slot[1], name))
        ins = e.h.dma_start(out=out, in_=in_, **kw)
        slot[1] += 16
        ins.then_inc(slot[0], 16)
        ev = (slot[0], slot[1], name)
        self._update(r, w, ev, name)
        return ev

    def barrier(self):
        evs = [(e.sem, e.count, e.name) for e in self.E.values() if e.count > 0]
        for q, sl in self.slots.items():
            for i, s in enumerate(sl):
                if s[1] > 0:
                    evs.append((s[0], s[1], "dma_%s%d" % (q, i)))
        for e in self.E.values():
            for ev in evs:
                if ev[2] == e.name:
                    continue
                e.wait(ev)
        self.trk = {}

    def sb(self, es, name, shape, dt):
        self.uid = getattr(self, "uid", 0) + 1
        return es.enter_context(self.nc.sbuf_tensor("%s_%d" % (name, self.uid), list(shape), dt))

    def dbg(self, name, ap, shape, dt=F32, r=()):
        if self.debug is None or name not in self.debug:
            return
        t = self.nc.dram_tensor("dbg_" + name, list(shape), dt, kind="ExternalOutput").ap()
        ev = self.dma("sp", t, ap, r=r)
        self.out_events.append(ev)
        self.dbg_outs[name] = "dbg_" + name


def build(debug=None, stop_after=None, small_moe=False):
    k = KB()
    k.debug = debug
    nc = k.nc
    es = k.es
    op = k.op
    dma = k.dma
    ps = k.ps

    def din(name, shape, dt=F32):
        return nc.dram_tensor(name, list(shape), dt, kind="ExternalInput").ap()

    x_in = din("x", [S, D])
    cT_in = din("cT", [128, 8])
    pos_in = din("pos", [1, S], I32)
    wb_in = [din("wb0", [40, 128, 1024]), din("wb1", [40, 128, 1024])]
    wo_in = din("wo", [2, D, D])
    ag_in = din("ag", [2, 512])
    rgain_in = din("rgain", [2, 512])
    lbl_in = din("lbl", [16, 128])
    adaw_in = din("adaw", [4, D, 3 * D])
    adab_in = din("adab", [4, 3 * D])
    lng_in = din("lng", [4, D])
    lnb_in = din("lnb", [4, D])
    fwg_in = din("fwg", [D, DFF])
    fwu_in = din("fwu", [D, DFF])
    fwd_in = din("fwd", [DFF, D])
    mr_in = din("mr", [D, NE])
    if small_moe:
        mwg_in = din("mwg", [1, 128, 128]); mwu_in = din("mwu", [1, 128, 128]); mwd_in = din("mwd", [1, 128, 128])
    else:
        mwg_in = din("mwg", [NE, D, DFE])
        mwu_in = din("mwu", [NE, D, DFE])
        mwd_in = din("mwd", [NE, DFE, D])
    y_out = nc.dram_tensor("y", [S, D], F32, kind="ExternalOutput").ap()
    xsA = nc.dram_tensor("xsA", [S, D], F32, kind="Internal").ap()
    xsB = nc.dram_tensor("xsB", [S, D], F32, kind="Internal").ap()

    sb = k.sb
    ones_f = sb(es, "ones_f", [128, 512], F32)
    ident_f = sb(es, "ident_f", [128, 128], F32)
    ident_b = sb(es, "ident_b", [128, 128], BF16)
    strip = sb(es, "strip", [128, STRW], BF16)
    mu = sb(es, "mu", [128, 64], F32)
    ml = sb(es, "ml", [128, 64], F32)
    mui = sb(es, "mui", [128, 8, 64], mybir.dt.uint8)
    mli = sb(es, "mli", [128, 8, 64], mybir.dt.uint8)
    rope_tab = nc.dram_tensor("rope_tab", [2, 128, S], F32, kind="Internal").ap()
    lb = sb(es, "lb", [128, 16], F32)
    oml = sb(es, "oml", [128, 16], F32)
    adac = sb(es, "adac", [128, 4, 16], F32)
    grow_d = nc.dram_tensor("grow_d", [4, 128, D], F32, kind="Internal").ap()
    cact = sb(es, "cact", [128, 8], F32)
    one11 = ones_f[0:1, 0:1]
    eps_rms = sb(es, "eps_rms", [128, 1], F32)
    eps_ln = sb(es, "eps_ln", [128, 1], F32)
    op("dve", lambda h: h.memset(eps_rms[:], RMS_EPS), w=["eps_rms"])
    op("dve", lambda h: h.memset(eps_ln[:], LN_EPS), w=["eps_ln"])
    nsh = sb(es, "nsh", [128, 1], F32)
    zero1 = sb(es, "zero1", [128, 1], F32)
    op("dve", lambda h: h.memset(nsh[:], -KSH), w=["nsh"])
    op("dve", lambda h: h.memset(zero1[:], 0.0), w=["zero1"])

    op("dve", lambda h: h.memset(ones_f[:], 1.0), w=["ones_f"])
    op("pool", lambda h: h.affine_select(out=ident_f[:], in_=ones_f[:, 0:128], pattern=[[-1, 128]],
                                          compare_op=ALU.is_equal, fill=0.0, base=0, channel_multiplier=1),
       r=["ones_f"], w=["ident_f"])
    op("dve", lambda h: h.tensor_copy(out=ident_b[:], in_=ident_f[:]), r=["ident_f"], w=["ident_b"])
    for half in range(2):
        sl = slice(half * 64, half * 64 + 64)
        op("pool", lambda h: h.affine_select(out=mu[sl, :], in_=ones_f[sl, 0:64], pattern=[[1, 64]],
                                              compare_op=ALU.is_ge, fill=0.0, base=0, channel_multiplier=-1),
           r=["ones_f"], w=["mu"])
        op("pool", lambda h: h.affine_select(out=ml[sl, :], in_=ones_f[sl, 0:64], pattern=[[-1, 64]],
                                              compare_op=ALU.is_ge, fill=0.0, base=0, channel_multiplier=1),
           r=["ones_f"], w=["ml"])

    for t8 in range(8):
        op("dve", lambda h: h.tensor_copy(out=mui[:, t8, :], in_=mu[:]), r=["mu"], w=["mui"])
        op("dve", lambda h: h.tensor_copy(out=mli[:, t8, :], in_=ml[:]), r=["ml"], w=["mli"])
    with ExitStack() as ts:
        vi = sb(ts, "vi", [128, STRW], I32)
        vj = sb(ts, "vj", [128, STRW], I32)
        vf = sb(ts, "vf", [128, STRW], F32)
        t1 = sb(ts, "t1", [128, STRW], F32)
        t2 = sb(ts, "t2", [128, STRW], F32)
        acc = sb(ts, "acc", [128, STRW], F32)
        op("pool", lambda h: h.iota(vi[:], pattern=[[-1, STRW]], base=C0, channel_multiplier=1), w=["vi"])
        op("dve", lambda h: h.tensor_copy(out=vf[:], in_=vi[:]), r=["vi"], w=["vf"])
        def inrange(dst, key, lim):
            op("dve", lambda h: h.tensor_scalar(out=dst[:], in0=vf[:], scalar1=lim, scalar2=None, op0=ALU.is_le),
               r=["vf"], w=[key])
            op("dve", lambda h: h.scalar_tensor_tensor(out=dst[:], in0=vf[:], scalar=-lim, in1=dst[:],
                                                        op0=ALU.is_ge, op1=ALU.mult), r=["vf", key], w=[key])
        inrange(acc, "acc", 64.0)
        for msk, lim in ((3, 256.0), (15, 1024.0)):
            op("dve", lambda h: h.tensor_scalar(out=vj[:], in0=vi[:], scalar1=msk, scalar2=None,
                                                 op0=ALU.bitwise_and), r=["vi"], w=["vj"])
            op("dve", lambda h: h.tensor_copy(out=t1[:], in_=vj[:]), r=["vj"], w=["t1"])
            op("dve", lambda h: h.tensor_scalar(out=t1[:], in0=t1[:], scalar1=0.0, scalar2=None,
                                                 op0=ALU.is_equal), r=["t1"], w=["t1"])
            inrange(t2, "t2", lim)
            op("dve", lambda h: h.tensor_tensor(out=t1[:], in0=t1[:], in1=t2[:], op=ALU.mult),
               r=["t1", "t2"], w=["t1"])
            op("dve", lambda h: h.tensor_tensor(out=acc[:], in0=acc[:], in1=t1[:], op=ALU.add),
               r=["acc", "t1"], w=["acc"])
        op("dve", lambda h: h.tensor_copy(out=strip[:], in_=acc[:]), r=["acc"], w=["strip"])
        k.dbg("strip", acc[:], [128, STRW], r=["acc"])
        k.barrier()

    with ExitStack() as ts:
        posi = sb(ts, "posi", [128, S], I32)
        ang = sb(ts, "ang", [128, S], F32)
        kf = sb(ts, "kf", [128, S], F32)
        ki = sb(ts, "ki", [128, S], I32)
        rr = sb(ts, "rr", [128, S], F32)
        mm = sb(ts, "mm", [128, S], F32)
        pidx = sb(ts, "pidx", [128, 1], I32)
        pj = sb(ts, "pj", [128, 1], I32)
        pf = sb(ts, "pf", [128, 1], F32)
        invf = sb(ts, "invf", [128, 1], F32)
        sgn = sb(ts, "sgn", [128, 1], F32)
        cosT = sb(ts, "cosT", [128, S], F32)
        sinT = sb(ts, "sinT", [128, S], F32)
        dma("sp", posi[:], pos_in[0:1, :].to_broadcast([128, S]), w=["posi"])
        op("pool", lambda h: h.iota(pidx[:], pattern=[[0, 1]], base=0, channel_multiplier=1), w=["pidx"])
        op("dve", lambda h: h.tensor_scalar(out=pj[:], in0=pidx[:], scalar1=31, scalar2=None, op0=ALU.bitwise_and),
           r=["pidx"], w=["pj"])
        op("dve", lambda h: h.tensor_copy(out=pf[:], in_=pj[:]), r=["pj"], w=["pf"])
        op("act", lambda h: h.activation(out=invf[:], in_=pf[:], func=AF.Exp, scale=-math.log(10000.0) / 32.0),
           r=["pf"], w=["invf"])
        op("dve", lambda h: h.tensor_scalar(out=pj[:], in0=pidx[:], scalar1=32, scalar2=None, op0=ALU.bitwise_and),
           r=["pidx", "pf"], w=["pj"])
        op("dve", lambda h: h.tensor_copy(out=pf[:], in_=pj[:]), r=["pj", "invf"], w=["pf"])
        op("dve", lambda h: h.tensor_scalar(out=sgn[:], in0=pf[:], scalar1=1.0 / 16.0, scalar2=-1.0,
                                             op0=ALU.mult, op1=ALU.add), r=["pf"], w=["sgn"])
        op("dve", lambda h: h.tensor_copy(out=ang[:], in_=posi[:]), r=["posi"], w=["ang"])
        op("dve", lambda h: h.tensor_scalar(out=ang[:], in0=ang[:], scalar1=invf[:, 0:1], scalar2=None,
                                             op0=ALU.mult), r=["ang", "invf"], w=["ang"])
        for which, dst in ((0, sinT), (1, cosT)):
            shift = 0.0 if which == 0 else math.pi / 2.0
            op("dve", lambda h: h.tensor_scalar(out=kf[:], in0=ang[:], scalar1=shift, scalar2=1.0 / TWO_PI,
                                                 op0=ALU.add, op1=ALU.mult), r=["ang"], w=["kf"])
            op("dve", lambda h: h.tensor_copy(out=ki[:], in_=kf[:]), r=["kf"], w=["ki"])
            op("dve", lambda h: h.tensor_copy(out=kf[:], in_=ki[:]), r=["ki"], w=["kf"])
            op("dve", lambda h: h.tensor_scalar(out=rr[:], in0=ang[:], scalar1=shift, scalar2=None, op0=ALU.add),
               r=["ang"], w=["rr"])
            op("dve", lambda h: h.scalar_tensor_tensor(out=rr[:], in0=kf[:], scalar=-CW1, in1=rr[:],
                                                        op0=ALU.mult, op1=ALU.add), r=["kf", "rr"], w=["rr"])
            op("dve", lambda h: h.scalar_tensor_tensor(out=rr[:], in0=kf[:], scalar=-CW2, in1=rr[:],
                                                        op0=ALU.mult, op1=ALU.add), r=["kf", "rr"], w=["rr"])
            op("dve", lambda h: h.tensor_scalar(out=mm[:], in0=rr[:], scalar1=math.pi, scalar2=None,
                                                 op0=ALU.is_gt), r=["rr"], w=["mm"])
            op("dve", lambda h: h.scalar_tensor_tensor(out=rr[:], in0=mm[:], scalar=-TWO_PI, in1=rr[:],
                                                        op0=ALU.mult, op1=ALU.add), r=["mm", "rr"], w=["rr"])
            op("dve", lambda h: h.tensor_scalar(out=mm[:], in0=rr[:], scalar1=-math.pi, scalar2=None,
                                                 op0=ALU.is_lt), r=["rr"], w=["mm"])
            op("dve", lambda h: h.scalar_tensor_tensor(out=rr[:], in0=mm[:], scalar=TWO_PI, in1=rr[:],
                                                        op0=ALU.mult, op1=ALU.add), r=["mm", "rr"], w=["rr"])
            op("dve", lambda h: h.tensor_scalar(out=rr[:], in0=rr[:], scalar1=-PI_SAFE, scalar2=PI_SAFE,
                                                 op0=ALU.max, op1=ALU.min), r=["rr"], w=["rr"])
            op("act", lambda h: h.activation(out=dst[:], in_=rr[:], func=AF.Sin), r=["rr"], w=["tab%d" % which])
        op("dve", lambda h: h.tensor_scalar(out=sinT[:], in0=sinT[:], scalar1=sgn[:, 0:1], scalar2=None,
                                             op0=ALU.mult), r=["tab0", "sgn"], w=["tab0"])
        dma("sp", rope_tab[0], cosT[:], r=["tab1"], w=["rope_tab0"])
        dma("sp", rope_tab[1], sinT[:], r=["tab0"], w=["rope_tab1"])
        k.dbg("cosT", cosT[:], [128, S], r=["tab1"])
        k.dbg("sinT", sinT[:], [128, S], r=["tab0"])
        k.barrier()

    with ExitStack() as ts:
        lrow = sb(ts, "lrow", [16, 128], F32)
        el = sb(ts, "el", [128, 16], F32)
        den = sb(ts, "den", [128, 8], F32)
        cT = sb(ts, "cT", [128, 8], F32)
        adarow = sb(ts, "adarow", [1, 3 * D], F32)
        adab = sb(ts, "adab", [1, 3 * D], F32)
        aw = [sb(ts, "aw%d" % i, [128, 8, 512], BF16) for i in range(4)]
        cact_b = sb(ts, "cact_b", [128, 8], BF16)
        grow = sb(ts, "grow", [128, 4, D], F32)
        dma("sp", lrow[:], lbl_in[:, :], w=["lrow"])
        op("pe", lambda h: h.transpose(out=ps[0][:, 0:16], in_=lrow[:], identity=ident_f[0:16, 0:16]),
           r=["lrow", "ident_f"], w=["ps0"])
        op("act", lambda h: h.activation(out=el[:], in_=ps[0][:, 0:16], func=AF.Exp), r=["ps0"], w=["el"])
        op("dve", lambda h: h.tensor_tensor(out=den[:], in0=el[:, 0:8], in1=el[:, 8:16], op=ALU.add),
           r=["el"], w=["den"])
        op("dve", lambda h: h.reciprocal(out=den[:], in_=den[:]), r=["den"], w=["den"])
        op("dve", lambda h: h.memset(lb[:, 0:8], 0.0), w=["lb"])
        op("dve", lambda h: h.tensor_tensor(out=lb[:, 8:16], in0=el[:, 8:16], in1=den[:], op=ALU.mult),
           r=["el", "den", "lb"], w=["lb"])
        op("dve", lambda h: h.tensor_scalar(out=oml[:], in0=lb[:], scalar1=-1.0, scalar2=1.0, op0=ALU.mult,
                                             op1=ALU.add), r=["lb"], w=["oml"])
        k.dbg("lb", lb[:], [128, 16], r=["lb"])
        dma("sp", cT[:], cT_in[:, :], w=["cT"])
        op("act", lambda h: h.activation(out=cact[:], in_=cT[:], func=AF.Silu), r=["cT"], w=["cact"])
        op("dve", lambda h: h.tensor_copy(out=cact_b[:], in_=cact[:]), r=["cact"], w=["cact_b"])
        n_aw = 0
        for ls in range(4):
            dma("sp", adab[:], adab_in[ls:ls + 1, :], w=["adab"])
            for cg in range(6):
                buf = aw[n_aw % 4]
                bk = "aw%d" % (n_aw % 4)
                n_aw += 1
                dma("pool", buf[:], adaw_in[ls].rearrange("(k p) f -> p k f", p=128)[:, :, cg * 512:(cg + 1) * 512],
                    w=[bk])
                for kk in range(8):
                    op("pe", lambda h: h.matmul(ps[1][0:1, :], lhsT=cact_b[:, kk:kk + 1], rhs=buf[:, kk, :],
                                                start=(kk == 0), stop=(kk == 7)),
                       r=[bk, "cact_b"], w=["ps1"])
                op("dve", lambda h: h.tensor_tensor(out=adarow[0:1, cg * 512:(cg + 1) * 512], in0=ps[1][0:1, :],
                                                     in1=adab[0:1, cg * 512:(cg + 1) * 512], op=ALU.add),
                   r=["ps1", "adab"], w=["adarow"])
            for j in range(16):
                op("pe", lambda h: h.matmul(ps[2][:, j:j + 1], lhsT=adarow[0:1, j * 128:(j + 1) * 128], rhs=one11,
                                            start=True, stop=True), r=["adarow", "ones_f"], w=["ps2"])
            op("dve", lambda h: h.tensor_copy(out=adac[:, ls, :], in_=ps[2][:, 0:16]), r=["ps2"], w=["adac"])
            op("dve", lambda h: h.tensor_scalar(out=adac[:, ls, 8:16], in0=adac[:, ls, 8:16], scalar1=1.0,
                                                 scalar2=None, op0=ALU.add), r=["adac"], w=["adac"])
            for half in range(2):
                op("pe", lambda h: h.matmul(ps[3][:, :], lhsT=ones_f[0:1, 0:128],
                                            rhs=adarow[0:1, 2048 + half * 512:2048 + (half + 1) * 512],
                                            start=True, stop=True), r=["adarow", "ones_f"], w=["ps3"])
                op("dve", lambda h: h.tensor_scalar(out=grow[:, ls, half * 512:(half + 1) * 512], in0=ps[3][:, :],
                                                     scalar1=1.0, scalar2=None, op0=ALU.add), r=["ps3"], w=["grow"])
        for ls in range(4):
            dma("sp", grow_d[ls], grow[:, ls, :], r=["grow"], w=[("grow_d", ls)])
        k.dbg("adac", adac[:], [128, 4, 16], r=["adac"])
        k.dbg("grow", grow[:], [128, 4, D], r=["grow"])
        k.barrier()

    if stop_after == "consts":
        return finish(k, y_out)
    C = dict(locals())
    for layer in range(2):
        src = x_in if layer == 0 else xsB
        if mixer(k, C, layer, src, xsA, stop=(stop_after[3:] if stop_after and stop_after.startswith("m%d_" % layer) else None)):
            return finish(k, y_out)
        k.dbg("x_mix%d" % layer, xsA, [S, D])
        if stop_after == "mix%d" % layer:
            return finish(k, y_out)
        ffn(k, C, layer, xsA, xsB if layer == 0 else y_out, is_final=(layer == 1))
        if layer == 0:
            k.dbg("x_ffn0", xsB, [S, D])
        if stop_after == "ffn%d" % layer:
            return finish(k, y_out)
    return finish(k, y_out)


def finish(k, y_out):
    e = k.E["sp"]
    for ev in k.out_events:
        e.wait(ev)
    k.es.close()
    return k


def _perm_swap():
    idx = []
    for h in range(8):
        idx += list(range(h * 64 + 32, h * 64 + 64)) + list(range(h * 64, h * 64 + 32))
    return np.array(idx)


def prep_shared(inp):
    f = lambda a: np.ascontiguousarray(np.asarray(a, dtype=np.float32))
    w_in = np.asarray(inp["w_in"], dtype=np.float32)
    p = _perm_swap()
    sh = {}
    for l in range(2):
        w = w_in[l]
        ext = np.concatenate([w[:, 0:512], w[:, 0:512][:, p], w[:, 512:1024], w[:, 512:1024][:, p],
                              w[:, 1024:]], axis=1)
        blk = ext.reshape(8, 128, 40, 128).transpose(2, 1, 0, 3).reshape(40, 128, 1024)
        sh["wb%d" % l] = np.ascontiguousarray(blk)
    sh["wo"] = f(inp["w_out"])
    sh["ag"] = f(inp["attn_norm_gain"])
    sh["rgain"] = f(inp["rec_norm_gain"])
    sh["lbl"] = f(inp["rec_lb_logits"]).reshape(16, 128)
    sh["adaw"] = f(inp["ada_w"]).reshape(4, D, 3 * D)
    sh["adab"] = f(inp["ada_b"]).reshape(4, 3 * D)
    sh["lng"] = f(inp["ln_gain"]).reshape(4, D)
    sh["lnb"] = f(inp["ln_bias"]).reshape(4, D)
    sh["fwg"] = f(inp["ffn_w_gate"])[0]
    sh["fwu"] = f(inp["ffn_w_up"])[0]
    sh["fwd"] = f(inp["ffn_w_down"])[0]
    sh["mr"] = f(inp["moe_router"])[0]
    sh["mwg"] = f(inp["moe_w_gate"])[0]
    sh["mwu"] = f(inp["moe_w_up"])[0]
    sh["mwd"] = f(inp["moe_w_down"])[0]
    return sh


def prep_core(inp, b):
    return {
        "x": np.ascontiguousarray(np.asarray(inp["x"][b], dtype=np.float32)),
        "cT": np.ascontiguousarray(np.asarray(inp["c"][b], dtype=np.float32).reshape(8, 128).T),
        "pos": np.ascontiguousarray(np.asarray(inp["positions"][b], dtype=np.int32).reshape(1, S)),
    }


def kernel(**inputs):
    sh = prep_shared(inputs)
    kb = build()
    in_maps = []
    for b in range(8):
        m = dict(sh)
        m.update(prep_core(inputs, b))
        in_maps.append(m)
    res = run_bass_kernel_spmd(kb.nc, in_maps, core_ids=list(range(8)))
    return np.stack([np.asarray(r["y"], dtype=np.float32) for r in res.results], axis=0)


class WPool:
    def __init__(self, k, es, wb, n, tag):
        self.k = k
        self.wb = wb
        self.bufs = [k.sb(es, "wblk", [128, 8, 128], BF16) for _ in range(n)]
        self.keys = ["%s_wblk%d" % (tag, i) for i in range(n)]
        self.i = 0

    def load(self, j):
        i = self.i
        self.i = (i + 1) % len(self.bufs)
        self.k.dma("pool", self.bufs[i][:], self.wb[j].rearrange("p (k c) -> p k c", c=128), w=[self.keys[i]])
        return self.bufs[i], self.keys[i]


def build_ut(k, C, UT, utag, ls, x_src=None, X=None, router=None):
    op, dma, ps, sb = k.op, k.dma, k.ps, k.sb
    adac, ident_f = C["adac"], C["ident_f"]
    with ExitStack() as ts:
        if X is None:
            xst = [sb(ts, "xst", [128, 4, D], F32) for _ in range(2)]
        for g in range(4):
            if X is None:
                buf = xst[g % 2]
                bk = "%s_xst%d" % (utag, g % 2)
                dma("sp", buf[:], x_src[g * 512:(g + 1) * 512, :].rearrange("(t p) d -> p t d", p=128), w=[bk])
                rk = [bk]
                tile = lambda t: buf[:, t, :]
            else:
                rk = [("X", 4 * g + t, hf) for t in range(4) for hf in range(2)]
                tile = lambda t: X[:, 4 * g + t, :]
            for kk in range(8):
                pb = ps[kk % 2]
                pk = "ps%d" % (kk % 2)
                for t in range(4):
                    op("pe", lambda h: h.transpose(out=pb[:, t * 128:(t + 1) * 128],
                                                   in_=tile(t)[:, kk * 128:(kk + 1) * 128], identity=ident_f[:]),
                       r=rk + ["ident_f"], w=[pk])
                op("act", lambda h: h.activation(out=UT[:, kk, g * 512:(g + 1) * 512], in_=pb[:, :], func=AF.Identity,
                                                 scale=adac[:, ls, 8 + kk:9 + kk], bias=adac[:, ls, kk:kk + 1]),
                   r=[pk, "adac"], w=[(utag, kk, g)])
                if router is not None:
                    router(g, kk, pb, pk)


def ut_keys(utag, g):
    return [(utag, kk, g) for kk in range(8)]


def mixer(k, C, layer, x_src, x_dst, stop=None):
    nc, op, dma, ps, sb = k.nc, k.op, k.dma, k.ps, k.sb
    ones_f, ident_f, ident_b, strip = C["ones_f"], C["ident_f"], C["ident_b"], C["strip"]
    wb = C["wb_in"][layer]
    ls = layer * 2
    T = "m%d" % layer
    utag = T + "UT"
    with ExitStack() as ms:
        UT = sb(ms, "UT", [128, 8, S], BF16)
        CATT = sb(ms, "CATT", [128, 8, S], BF16)
        build_ut(k, C, UT, utag, ls, x_src=x_src)
        k.dbg("UT%d" % layer, UT[:], [128, 8, S], BF16, r=[(utag, kk, g) for kk in range(8) for g in range(4)])
        if stop == "ut":
            k.barrier()
            return True
        with ExitStack() as as_:
            QT = sb(as_, "QT", [128, 4, S], BF16)
            KT = sb(as_, "KT", [128, 4, S], BF16)
            VA = sb(as_, "VA", [128, NT, 8, 65], BF16)
            WV = sb(as_, "WV", [128, 4, 8, 128], BF16)
            zer = sb(as_, "zer", [128, 512], BF16)
            AG = sb(as_, "AG", [128, 512], F32)
            ta = [sb(as_, "ta", [128, 512], F32) for _ in range(2)]
            tb_ = [sb(as_, "tb", [128, 512], F32) for _ in range(2)]
            Eb = [sb(as_, "Eb", [128, 512], BF16) for _ in range(3)]
            Em = [sb(as_, "Em", [128, 512], BF16) for _ in range(3)]
            ATT = sb(as_, "ATT", [128, 4, 512], F32)
            ATB = [sb(as_, "ATB", [128, 512], BF16) for _ in range(2)]
            junk = sb(as_, "junk", [128, 512], F32)
            ss = sb(as_, "ss", [128, 4], F32)
            rcp = sb(as_, "rcp", [128, 4], F32)
            wp = WPool(k, as_, wb, 4, T + "a")
            cosT = sb(as_, "cosT", [128, S], F32)
            sinT = sb(as_, "sinT", [128, S], F32)
            dma("sp", cosT[:], C["rope_tab"][0], w=["cosT"])
            dma("sp", sinT[:], C["rope_tab"][1], w=["sinT"])
            op("dve", lambda h: h.memset(zer[:], 0.0), w=["zer"])
            op("dve", lambda h: h.memset(VA[:], 1.0), w=["VA"])
            dma("sp", AG[:], C["ag_in"][layer:layer + 1, :].to_broadcast([128, 512]), w=["AG"])
            it = 0
            for c in range(4):
                dma("pool", WV[:, c, :, :], wb[16 + c].rearrange("p (k c) -> p k c", c=128), w=[("WV", c)])
            qk_blocks = [(dst, dname, jq + c, jqs + c, c)
                         for (dst, dname, jq, jqs) in ((QT, "QT", 0, 4), (KT, "KT", 8, 12)) for c in range(4)]
            loaded = [(wp.load(qk_blocks[0][2]), wp.load(qk_blocks[0][3]))]
            for bi_, (dst, dname, _jq, _jqs, c) in enumerate(qk_blocks):
                if True:
                    if bi_ + 1 < len(qk_blocks):
                        loaded.append((wp.load(qk_blocks[bi_ + 1][2]), wp.load(qk_blocks[bi_ + 1][3])))
                    (Wq, kq), (Ws, ks_) = loaded[bi_]
                    for tb in range(4):
                        p1 = 2 + 2 * (it % 2)
                        p2 = p1 + 1
                        i2 = it % 2
                        it += 1
                        for kk in range(8):
                            op("pe", lambda h: h.matmul(ps[p1][:, :], lhsT=Wq[:, kk, :],
                                                        rhs=UT[:, kk, tb * 512:(tb + 1) * 512],
                                                        start=(kk == 0), stop=(kk == 7)),
                               r=[kq, (utag, kk, tb)], w=["ps%d" % p1])
                        for kk in range(8):
                            op("pe", lambda h: h.matmul(ps[p2][:, :], lhsT=Ws[:, kk, :],
                                                        rhs=UT[:, kk, tb * 512:(tb + 1) * 512],
                                                        start=(kk == 0), stop=(kk == 7)),
                               r=[ks_, (utag, kk, tb)], w=["ps%d" % p2])
                        op("dve", lambda h: h.tensor_tensor(out=ta[i2][:], in0=ps[p1][:, :],
                                                             in1=cosT[:, tb * 512:(tb + 1) * 512], op=ALU.mult),
                           r=["ps%d" % p1, "cosT"], w=["ta%d" % i2])
                        op("dve", lambda h: h.tensor_tensor(out=tb_[i2][:], in0=ps[p2][:, :],
                                                             in1=sinT[:, tb * 512:(tb + 1) * 512], op=ALU.mult),
                           r=["ps%d" % p2, "sinT"], w=["tb%d" % i2])
                        op("pool", lambda h: h.tensor_tensor(out=dst[:, c, tb * 512:(tb + 1) * 512], in0=ta[i2][:],
                                                              in1=tb_[i2][:], op=ALU.add),
                           r=["ta%d" % i2, "tb%d" % i2], w=[(dname, c, tb)])
            for t in range(NT):
                pv = 2 + (t % 2)
                for c in range(4):
                    for kk in range(8):
                        op("pe", lambda h: h.matmul(ps[pv][:, c * 128:(c + 1) * 128],
                                                    lhsT=UT[:, kk, t * 128:(t + 1) * 128], rhs=WV[:, c, kk, :],
                                                    start=(kk == 0), stop=(kk == 7)),
                           r=[("WV", c), (utag, kk, t // 4)], w=["ps%d" % pv])
                op("act", lambda h: h.activation(out=VA[:, t, :, 0:64],
                                                 in_=ps[pv][:, :].rearrange("p (h d) -> p h d", d=64),
                                                 func=AF.Copy), r=["ps%d" % pv, "VA"], w=[("VA", t)])
            if layer == 0:
                k.dbg("QT", QT[:], [128, 4, S], BF16, r=[("QT", c, tb) for c in range(4) for tb in range(4)])
                k.dbg("KT", KT[:], [128, 4, S], BF16, r=[("KT", c, tb) for c in range(4) for tb in range(4)])
            if stop == "qkv":
                k.barrier()
                return True
            items = []
            for qb in range(4):
                for hd in range(8):
                    kts = list(range(max(0, 4 * qb - 8), min(15, 4 * qb + 3 + 8) + 1))
                    for kt in kts:
                        items.append((qb, hd, kt, kt == kts[0], kt == kts[-1]))
            NI = len(items)
            PD = 3

            def emit_score(i):
                qb, hd, kt, _, _ = items[i]
                c = hd // 2
                pb = (hd % 2) * 64
                sbk = i % 4
                op("pe", lambda h: h.matmul(ps[sbk][:, :], lhsT=KT[pb:pb + 64, c, kt * 128:(kt + 1) * 128],
                                            rhs=QT[pb:pb + 64, c, qb * 512:(qb + 1) * 512], start=True, stop=True),
                   r=[("KT", c, kt // 4), ("QT", c, qb)], w=["ps%d" % sbk])

            def emit_rest(i):
                qb, hd, kt, isf, isl = items[i]
                sbk = i % 4
                ei = i % 3
                ob = 6 + (hd % 2)
                okey = "ps%d" % ob
                if isf:
                    op("pe", lambda h: h.matmul(ps[ob][:, 0:260], lhsT=zer[:, 0:128], rhs=zer[:, 0:260],
                                                start=True, stop=False, skip_group_check=True),
                       r=["zer"], w=[okey])
                op("act", lambda h: h.activation(out=Eb[ei][:], in_=ps[sbk][:, :], func=AF.Exp, scale=0.125),
                   r=["ps%d" % sbk], w=["Eb%d" % ei])
                cs = C0 - 128 * (kt - 4 * qb)
                meng = "dve" if (i % 4 != 3) else "pool"
                op(meng, lambda h: h.tensor_tensor(out=Em[ei][:], in0=Eb[ei][:], in1=strip[:, cs:cs + 512],
                                                   op=ALU.mult), r=["Eb%d" % ei], w=["Em%d" % ei])
                for j in range(4):
                    qt = 4 * qb + j
                    if abs(kt - qt) > 8:
                        continue
                    op("pe", lambda h: h.matmul(ps[ob][:, j * 65:(j + 1) * 65],
                                                lhsT=Em[ei][:, j * 128:(j + 1) * 128], rhs=VA[:, kt, hd, :],
                                                start=False, stop=(kt == min(15, qt + 8)), skip_group_check=True),
                       r=["Em%d" % ei, ("VA", kt)], w=[okey])
                if not isl:
                    return
                ov = ps[ob][:, 0:260].rearrange("p (j e) -> p j e", e=65)
                op("dve", lambda h: h.reciprocal(out=rcp[:], in_=ov[:, :, 64]), r=[okey], w=["rcp"])
                op("dve", lambda h: h.tensor_tensor(out=ATT[:, :, hd * 64:(hd + 1) * 64], in0=ov[:, :, 0:64],
                                                     in1=rcp[:].unsqueeze(2).to_broadcast([128, 4, 64]),
                                                     op=ALU.mult), r=[okey, "rcp"], w=[("ATT", hd)])
                if hd != 7:
                    return
                if layer == 0 and qb == 0:
                    k.dbg("ATT0", ATT[:], [128, 4, 512], r=[("ATT", h_) for h_ in range(8)])
                akeys = [("ATT", h_) for h_ in range(8)]
                for j in range(4):
                    op("act", lambda h: h.activation(out=junk[:], in_=ATT[:, j, :], func=AF.Square,
                                                     accum_out=ss[:, j:j + 1]), r=akeys, w=["junk", ("ss", j)])
                op("act", lambda h: h.activation(out=ss[:], in_=ss[:], func=AF.Sqrt, scale=1.0 / 512.0,
                                                 bias=C["eps_rms"][:, 0:1]),
                   r=[("ss", j) for j in range(4)], w=["ssq"])
                op("dve", lambda h: h.reciprocal(out=ss[:], in_=ss[:]), r=["ssq"], w=["ssr"])
                for j in range(4):
                    t = 4 * qb + j
                    a2 = j % 2
                    op("dve", lambda h: h.scalar_tensor_tensor(out=ATB[a2][:], in0=ATT[:, j, :], scalar=ss[:, j:j + 1],
                                                                in1=AG[:], op0=ALU.mult, op1=ALU.mult),
                       r=akeys + ["ssr", "AG"], w=["ATB%d" % a2])
                    pt = 4 + (j % 2)
                    pbf = ps[pt][:, :].bitcast(BF16)
                    for cc in range(4):
                        op("pe", lambda h: h.transpose(out=pbf[:, cc * 128:(cc + 1) * 128],
                                                       in_=ATB[a2][:, cc * 128:(cc + 1) * 128], identity=ident_b[:]),
                           r=["ATB%d" % a2, "ident_b"], w=["ps%d" % pt])
                    op("act", lambda h: h.activation(out=CATT[:, 0:4, t * 128:(t + 1) * 128],
                                                     in_=pbf[:, 0:512].rearrange("p (c t) -> p c t", t=128),
                                                     func=AF.Copy), r=["ps%d" % pt], w=[("CATT", 0, t)])

            for i in range(NI + PD):
                if i < NI:
                    emit_score(i)
                if i >= PD:
                    emit_rest(i - PD)
            k.barrier()
        if stop == "attn":
            return True
        with ExitStack() as rs:
            lb, oml, mu, ml = C["lb"], C["oml"], C["mui"], C["mli"]
            RI = sb(rs, "RI", [128, NT, 512], BF16)
            WR4 = sb(rs, "WR4", [128, 4, 8, 128], BF16)
            wp = WPool(k, rs, wb, 4, T + "r")
            RQ = sb(rs, "RQ", [128, S], BF16)
            FB = sb(rs, "FB", [128, S], F32)
            KK = sb(rs, "KK", [128, S], BF16)
            GX = sb(rs, "GX", [128, S + 1], F32)
            E1 = sb(rs, "E1", [128, S], BF16)
            E2 = sb(rs, "E2", [128, S], BF16)
            QD = sb(rs, "QD", [128, S], BF16)
            KD = sb(rs, "KD", [128, S], BF16)
            KTOK = sb(rs, "KTOK", [128, NT, 128], BF16)
            SC = sb(rs, "SC", [128, 3, 32], F32)
            AMA = [sb(rs, "AMA", [128, 2, NT, 64], BF16) for _ in range(2)]
            Sst = sb(rs, "Sst", [128, 128], F32)
            DSg = sb(rs, "DSg", [128, 32, 128], BF16)
            SAA = sb(rs, "SAA", [128, 32, 128], BF16)
            RECO = sb(rs, "RECO", [128, NT, 128], F32)
            RECN = sb(rs, "RECN", [128, NT, 512], BF16)
            rst = sb(rs, "rst", [128, NT], F32)
            RGN = sb(rs, "RGN", [128, 512], F32)
            SG = [sb(rs, "SG", [128, 512], F32) for _ in range(1)]
            RB = [sb(rs, "RB", [128, 512], BF16) for _ in range(1)]
            dma("sp", RGN[:], C["rgain_in"][layer:layer + 1, :].to_broadcast([128, 512]), w=["RGN"])
            zst = sb(rs, "zst", [128, 128], BF16)
            op("dve", lambda h: h.memset(zst[:], 0.0), w=["zst"])
            for d in range(2):
                op("dve", lambda h: h.memset(AMA[d][:], 0.0), w=[("AMA", d, 0), ("AMA", d, 1)])
            for c in range(4):
                dma("pool", WR4[:, c, :, :], wb[32 + c].rearrange("p (k c) -> p k c", c=128), w=[("WR4", c)])
            for t in range(NT):
                pv = t % 2
                for c in range(4):
                    for kk in range(8):
                        op("pe", lambda h: h.matmul(ps[pv][:, c * 128:(c + 1) * 128],
                                                    lhsT=UT[:, kk, t * 128:(t + 1) * 128], rhs=WR4[:, c, kk, :],
                                                    start=(kk == 0), stop=(kk == 7)),
                           r=[("WR4", c), (utag, kk, t // 4)], w=["ps%d" % pv])
                op("act", lambda h: h.activation(out=RI[:, t, :], in_=ps[pv][:, :], func=AF.Copy),
                   r=["ps%d" % pv], w=[("RI", t)])
            for c in range(4):
                dma("pool", WR4[:, c, :, :], wb[36 + c].rearrange("p (k c) -> p k c", c=128), w=[("WR4", c)])
            op("dve", lambda h: h.memset(GX[:, 0:1], 0.0), w=["GX"])
            nmm = 0
            for hh in range(4):
                Wq, kq = wp.load(20 + hh)
                for tb in range(4):
                    pb_ = nmm % 2
                    nmm += 1
                    for kk in range(8):
                        op("pe", lambda h: h.matmul(ps[pb_][:, :], lhsT=Wq[:, kk, :],
                                                    rhs=UT[:, kk, tb * 512:(tb + 1) * 512],
                                                    start=(kk == 0), stop=(kk == 7)),
                           r=[kq, (utag, kk, tb)], w=["ps%d" % pb_])
                    op("act", lambda h: h.activation(out=RQ[:, tb * 512:(tb + 1) * 512], in_=ps[pb_][:, :],
                                                     func=AF.Silu), r=["ps%d" % pb_], w=["RQ"])
                for d in range(2):
                    Wf, kf_ = wp.load(24 + 4 * d + hh)
                    col = layer * 8 + d * 4 + hh
                    for tb in range(4):
                        pb_ = nmm % 2
                        nmm += 1
                        for kk in range(8):
                            op("pe", lambda h: h.matmul(ps[pb_][:, :], lhsT=Wf[:, kk, :],
                                                        rhs=UT[:, kk, tb * 512:(tb + 1) * 512],
                                                        start=(kk == 0), stop=(kk == 7)),
                               r=[kf_, (utag, kk, tb)], w=["ps%d" % pb_])
                        op("act", lambda h: h.activation(out=FB[:, tb * 512:(tb + 1) * 512], in_=ps[pb_][:, :],
                                                         func=AF.Sigmoid), r=["ps%d" % pb_], w=["FB"])
                    op("dve", lambda h: h.tensor_scalar(out=FB[:], in0=FB[:], scalar1=oml[:, col:col + 1],
                                                         scalar2=lb[:, col:col + 1], op0=ALU.mult, op1=ALU.add),
                       r=["FB", "lb", "oml"], w=["FB"])
                    op("dve", lambda h: h.tensor_scalar(out=KK[:], in0=FB[:], scalar1=-1.0, scalar2=1.0,
                                                         op0=ALU.mult, op1=ALU.add), r=["FB"], w=["KK"])
                    op("act", lambda h: h.activation(out=FB[:], in_=FB[:], func=AF.Ln), r=["FB", "KK"], w=["FB"])
                    op("dve", lambda h: h.tensor_tensor_scan(out=GX[:, 1:S + 1],
                                                              data0=ones_f[:, 0:1].to_broadcast([128, S]),
                                                              data1=FB[:], initial=0.0, op0=ALU.mult, op1=ALU.add),
                       r=["FB", "ones_f"], w=["GX"])
                    if layer == 0 and hh == 0:
                        k.dbg("GX%d" % d, GX[:], [128, S + 1], r=["GX"])
                    s0 = GX[:, 0:S:64]
                    sm = GX[:, 32:S + 1:64]
                    s1 = GX[:, 64:S + 1:64]
                    tok = GX[:, 1:S + 1] if d == 0 else GX[:, 0:S]
                    op("dve", lambda h: h.tensor_tensor(out=FB[:].rearrange("p (n t) -> p n t", t=64),
                                                         in0=tok.rearrange("p (n t) -> p n t", t=64),
                                                         in1=sm.unsqueeze(2).to_broadcast([128, 32, 64]),
                                                         op=ALU.subtract), r=["GX", "FB"], w=["FB"])
                    b1 = C["nsh"][:, 0:1] if d == 1 else C["zero1"][:, 0:1]
                    b2 = C["nsh"][:, 0:1] if d == 0 else C["zero1"][:, 0:1]
                    op("act", lambda h: h.activation(out=E1[:], in_=FB[:], func=AF.Exp, bias=b1), r=["FB"], w=["E1"])
                    op("act", lambda h: h.activation(out=E2[:], in_=FB[:], func=AF.Exp, scale=-1.0, bias=b2),
                       r=["FB"], w=["E2"])
                    eq, ek = (E1, E2) if d == 0 else (E2, E1)
                    op("dve", lambda h: h.tensor_tensor(out=QD[:], in0=RQ[:], in1=eq[:], op=ALU.mult),
                       r=["RQ", "E1", "E2"], w=["QD"])
                    op("dve", lambda h: h.tensor_tensor(out=KD[:], in0=KK[:], in1=ek[:], op=ALU.mult),
                       r=["KK", "E1", "E2"], w=["KD"])
                    for idx, (a_, b_) in enumerate(((sm, s0), (s1, s0), (s1, sm))):
                        op("dve", lambda h: h.tensor_tensor(out=SC[:, idx, :], in0=a_, in1=b_, op=ALU.subtract),
                           r=["GX", "SCe"], w=[("SC", idx)])
                    op("act", lambda h: h.activation(out=SC[:], in_=SC[:], func=AF.Exp),
                       r=[("SC", i_) for i_ in range(3)], w=["SCe"] + [("SC", i_) for i_ in range(3)])
                    for t4 in range(4):
                        pbf = ps[pb_ := (nmm % 2)][:, :].bitcast(BF16)
                        nmm += 1
                        for tt in range(4):
                            t = 4 * t4 + tt
                            op("pe", lambda h: h.transpose(out=pbf[:, tt * 128:(tt + 1) * 128],
                                                           in_=KD[:, t * 128:(t + 1) * 128], identity=ident_b[:]),
                               r=["KD", "ident_b"], w=["ps%d" % pb_])
                        op("act", lambda h: h.activation(out=KTOK[:, 4 * t4:4 * t4 + 4, :],
                                                         in_=pbf[:, 0:512].rearrange("p (c t) -> p c t", t=128),
                                                         func=AF.Copy), r=["ps%d" % pb_], w=["KTOK"])
                    ia, ig = (0, 2) if d == 0 else (2, 0)
                    msk = mu if d == 0 else ml
                    hc = slice(hh * 128, (hh + 1) * 128)
                    for bb in range(2):
                        bank = 2 + bb
                        for t8 in range(8):
                            t = bb * 8 + t8
                            for hf in range(2):
                                n = 2 * t + hf
                                po = hf * 64
                                tk = slice(n * 64, n * 64 + 64)
                                op("pe", lambda h: h.matmul(ps[bank][po:po + 64, t8 * 64:(t8 + 1) * 64],
                                                            lhsT=KD[:, tk], rhs=QD[:, tk], start=True, stop=True),
                                   r=["KD", "QD"], w=["ps%d" % bank])
                        for hf in range(2):
                            hs = slice(hf * 64, (hf + 1) * 64)
                            op("dve", lambda h: h.copy_predicated(
                                out=AMA[d][hs, hf, bb * 8:(bb + 1) * 8, :],
                                mask=msk[hs, :, :],
                                data=ps[bank][hs, :].rearrange("p (t s) -> p t s", s=64)),
                               r=["ps%d" % bank, "mui", "mli"], w=[("AMA", d, bb)])
                    for Q in range(4):
                        bk0 = 4 if Q % 2 == 0 else 2
                        for c8 in range(8):
                            n = 8 * Q + c8
                            t = n // 2
                            po = (n % 2) * 64
                            bank = bk0 + (n % 2)
                            cc = c8 // 2
                            op("pe", lambda h: h.matmul(ps[bank][:, cc * 128:(cc + 1) * 128],
                                                        lhsT=KTOK[po:po + 64, t, :], rhs=RI[po:po + 64, t, hc],
                                                        start=True, stop=True),
                               r=["KTOK", ("RI", t)], w=["ps%d" % bank])
                        for par in range(2):
                            bank = bk0 + par
                            lo = 8 * Q + par
                            op("dve", lambda h: h.tensor_tensor(
                                out=DSg[:, lo:lo + 7:2, :], in0=ps[bank][:, :].rearrange("p (c v) -> p c v", v=128),
                                in1=SC[:, ig, lo:lo + 7:2].unsqueeze(2).to_broadcast([128, 4, 128]), op=ALU.mult),
                               r=["ps%d" % bank, "SCe"], w=[("DSg", Q)])
                    order = list(range(32)) if d == 0 else list(range(31, -1, -1))
                    prev = {order[i + 1]: order[i] for i in range(31)}
                    op("dve", lambda h: h.tensor_tensor(out=QD[:].rearrange("p (n t) -> p n t", t=64),
                                                         in0=QD[:].rearrange("p (n t) -> p n t", t=64),
                                                         in1=SC[:, ia, :].unsqueeze(2).to_broadcast([128, 32, 64]),
                                                         op=ALU.mult), r=["QD", "SCe"], w=["QD"])
                    for i, n in enumerate(order[:-1]):
                        if i == 0:
                            op("dve", lambda h: h.tensor_copy(out=SAA[:, n, :], in_=DSg[:, n, :]),
                               r=[("DSg", n // 8)], w=[("SAA", n)])
                        else:
                            p_ = prev[n]
                            op("dve", lambda h: h.scalar_tensor_tensor(out=SAA[:, n, :], in0=SAA[:, p_, :],
                                                                        scalar=SC[:, 1, n:n + 1], in1=DSg[:, n, :],
                                                                        op0=ALU.mult, op1=ALU.add),
                               r=[("SAA", p_), ("DSg", n // 8), "SCe"], w=[("SAA", n)])
                    qorder = list(range(4)) if d == 0 else list(range(3, -1, -1))
                    for q in qorder:
                        bank = 6 + (q % 2)
                        for t4 in range(4):
                            t = 4 * q + t4
                            for hf in range(2):
                                n = 2 * t + hf
                                po = hf * 64
                                tk = slice(n * 64, n * 64 + 64)
                                firstn = (n == order[0])
                                op("pe", lambda h: h.matmul(ps[bank][po:po + 64, t4 * 128:(t4 + 1) * 128],
                                                            lhsT=AMA[d][:, hf, t, :], rhs=RI[:, t, hc],
                                                            start=True, stop=False),
                                   r=[("AMA", d, t // 8), ("RI", t)], w=["ps%d" % bank])
                                if n == order[0]:
                                    st_ap, st_key = zst[:, :], "zst"
                                else:
                                    st_ap, st_key = SAA[:, prev[n], :], ("SAA", prev[n])
                                op("pe", lambda h: h.matmul(ps[bank][po:po + 64, t4 * 128:(t4 + 1) * 128],
                                                            lhsT=QD[:, tk], rhs=st_ap,
                                                            start=False, stop=True),
                                   r=["QD", st_key], w=["ps%d" % bank])
                        pv_ = ps[bank][:, :].rearrange("p (t v) -> p t v", v=128)
                        if d == 0:
                            op("act", lambda h: h.activation(out=RECO[:, 4 * q:4 * q + 4, :], in_=pv_, func=AF.Copy,
                                                             scale=math.exp(KSH)),
                               r=["ps%d" % bank], w=[("RECO", q)])
                        else:
                            op("dve", lambda h: h.scalar_tensor_tensor(out=RECO[:, 4 * q:4 * q + 4, :], in0=pv_,
                                                                        scalar=math.exp(KSH),
                                                                        in1=RECO[:, 4 * q:4 * q + 4, :],
                                                                        op0=ALU.mult, op1=ALU.add),
                               r=["ps%d" % bank, ("RECO", q)], w=[("RECO", q)])
                rkeys = [("RECO", q) for q in range(4)]
                if layer == 0 and hh == 0:
                    k.dbg("RECO0", RECO[:], [128, NT, 128], r=rkeys)
                fb3 = FB[:].rearrange("p (t v) -> p t v", v=128)
                op("dve", lambda h: h.tensor_tensor(out=fb3, in0=RECO[:], in1=RECO[:], op=ALU.mult),
                   r=rkeys + ["FB"], w=["FB"])
                op("dve", lambda h: h.tensor_reduce(out=rst[:], in_=fb3, axis=AX.X, op=ALU.add), r=["FB"], w=["rst"])
                op("act", lambda h: h.activation(out=rst[:], in_=rst[:], func=AF.Sqrt, scale=1.0 / 128.0,
                                                 bias=C["eps_rms"][:, 0:1]), r=["rst"], w=["rst"])
                op("dve", lambda h: h.reciprocal(out=rst[:], in_=rst[:]), r=["rst"], w=["rst"])
                op("dve", lambda h: h.tensor_tensor(out=fb3, in0=RECO[:],
                                                     in1=rst[:].unsqueeze(2).to_broadcast([128, NT, 128]),
                                                     op=ALU.mult), r=rkeys + ["rst", "FB"], w=["FB"])
                op("dve", lambda h: h.tensor_tensor(out=RECN[:, :, hh * 128:(hh + 1) * 128], in0=fb3,
                                                     in1=RGN[:, hh * 128:(hh + 1) * 128].unsqueeze(1)
                                                     .to_broadcast([128, NT, 128]), op=ALU.mult),
                   r=["FB", "RGN"] + rkeys, w=[("RECN", hh)])
            for t in range(NT):
                pv = t % 2
                i2 = 0
                for c in range(4):
                    for kk in range(8):
                        op("pe", lambda h: h.matmul(ps[pv][:, c * 128:(c + 1) * 128],
                                                    lhsT=UT[:, kk, t * 128:(t + 1) * 128], rhs=WR4[:, c, kk, :],
                                                    start=(kk == 0), stop=(kk == 7)),
                           r=[("WR4", c), (utag, kk, t // 4)], w=["ps%d" % pv])
                op("act", lambda h: h.activation(out=SG[i2][:], in_=ps[pv][:, :], func=AF.Sigmoid),
                   r=["ps%d" % pv], w=["SG%d" % i2])
                op("dve", lambda h: h.tensor_tensor(out=RB[i2][:], in0=RECN[:, t, :], in1=SG[i2][:], op=ALU.mult),
                   r=["SG%d" % i2] + [("RECN", hh) for hh in range(4)], w=["RB%d" % i2])
                pt = 2 + (t % 2)
                pbf = ps[pt][:, :].bitcast(BF16)
                for cc in range(4):
                    op("pe", lambda h: h.transpose(out=pbf[:, cc * 128:(cc + 1) * 128],
                                                   in_=RB[i2][:, cc * 128:(cc + 1) * 128], identity=ident_b[:]),
                       r=["RB%d" % i2, "ident_b"], w=["ps%d" % pt])
                op("act", lambda h: h.activation(out=CATT[:, 4:8, t * 128:(t + 1) * 128],
                                                 in_=pbf[:, 0:512].rearrange("p (c t) -> p c t", t=128),
                                                 func=AF.Copy), r=["ps%d" % pt], w=[("CATT", 1, t)])
            k.barrier()
        if stop == "rec":
            return True
        with ExitStack() as os_:
            WO = sb(os_, "WO", [128, 8, D], BF16)
            for kk in range(8):
                dma("pool", WO[:, kk, :], C["wo_in"][layer, kk * 128:(kk + 1) * 128, :], w=[("WO", kk)])
            wkeys = [("WO", kk) for kk in range(8)]

            def ymm(t, half, bank, bkey):
                for kk in range(8):
                    op("pe", lambda h: h.matmul(ps[bank][:, :], lhsT=CATT[:, kk, t * 128:(t + 1) * 128],
                                                rhs=WO[:, kk, half * 512:(half + 1) * 512],
                                                start=(kk == 0), stop=(kk == 7)), r=wkeys, w=[bkey])
            resid_ln(k, C, os_, ls, ymm, x_src=x_src, x_dst=x_dst, tag=T + "o")
            k.barrier()


def resid_ln(k, C, es, ls, ymm, x_src, x_dst, tag, X=None):
    op, dma, ps, sb = k.op, k.dma, k.ps, k.sb
    if X is None:
        grow = sb(es, "grow", [128, D], F32)
        dma("sp", grow[:], C["grow_d"][ls], w=["grow"])
    LNG = sb(es, "LNG", [128, D], F32)
    LNB = sb(es, "LNB", [128, D], F32)
    dma("sp", LNG[:], C["lng_in"][ls:ls + 1, :].to_broadcast([128, D]), w=[tag + "LNG"])
    dma("sp", LNB[:], C["lnb_in"][ls:ls + 1, :].to_broadcast([128, D]), w=[tag + "LNB"])
    NB = 3
    xt = [sb(es, "xt", [128, D], F32) for _ in range(NB)]
    zt = [sb(es, "zt", [128, D], F32) for _ in range(NB)] if X is None else None
    st = [sb(es, "st", [128, 2, 6], F32) for _ in range(NB)]
    mv = [sb(es, "mv", [128, 2], F32) for _ in range(NB)]
    rs_ = [sb(es, "rs", [128, 2], F32) for _ in range(NB)]
    for t in range(NT):
        i2 = t % NB
        zk = tag + "zt%d" % i2
        if X is None:
            xk = tag + "xt%d" % i2
            dma("sp", xt[i2][:], x_src[t * 128:(t + 1) * 128, :], w=[xk])
            for half in range(2):
                bank = 2 + 2 * i2 + half
                bkey = "ps%d" % bank
                ymm(t, half, bank, bkey)
                op("dve", lambda h: h.tensor_tensor(out=zt[i2][:, half * 512:(half + 1) * 512], in0=ps[bank][:, :],
                                                     in1=grow[:, half * 512:(half + 1) * 512], op=ALU.mult),
                   r=[bkey, "grow"], w=[zk])
            op("dve", lambda h: h.scalar_tensor_tensor(out=zt[i2][:], in0=xt[i2][:], scalar=ALPHA, in1=zt[i2][:],
                                                        op0=ALU.mult, op1=ALU.add), r=[xk, zk], w=[zk])
            z = zt[i2][:, :]
            zkeys = [zk]
        else:
            z = X[:, t, :]
            zkeys = [("X", t, 0), ("X", t, 1)]
        for half in range(2):
            op("dve", lambda h: h.bn_stats(out=st[i2][:, half, :], in_=z[:, half * 512:(half + 1) * 512]),
               r=zkeys, w=[tag + "st%d" % i2])
        op("dve", lambda h: h.bn_aggr(out=mv[i2][:], in_=st[i2][:].rearrange("p a b -> p (a b)")),
           r=[tag + "st%d" % i2], w=[tag + "mv%d" % i2])
        op("act", lambda h: h.activation(out=rs_[i2][:, 0:1], in_=mv[i2][:, 1:2], func=AF.Sqrt,
                                         bias=C["eps_ln"][:, 0:1]), r=[tag + "mv%d" % i2], w=[tag + "rs%d" % i2])
        op("dve", lambda h: h.reciprocal(out=rs_[i2][:, 0:1], in_=rs_[i2][:, 0:1]),
           r=[tag + "rs%d" % i2], w=[tag + "rs%d" % i2])
        op("dve", lambda h: h.scalar_tensor_tensor(out=rs_[i2][:, 1:2], in0=mv[i2][:, 0:1], scalar=-1.0,
                                                    in1=rs_[i2][:, 0:1], op0=ALU.mult, op1=ALU.mult),
           r=[tag + "mv%d" % i2, tag + "rs%d" % i2], w=[tag + "rs%d" % i2])
        ok = tag + "on%d" % i2
        o = xt[i2]
        op("act", lambda h: h.activation(out=o[:], in_=z, func=AF.Identity, scale=rs_[i2][:, 0:1],
                                         bias=rs_[i2][:, 1:2]),
           r=zkeys + [tag + "rs%d" % i2, tag + "xt%d" % i2], w=[ok, tag + "xt%d" % i2])
        op("dve", lambda h: h.tensor_tensor(out=o[:], in0=o[:], in1=LNG[:], op=ALU.mult),
           r=[ok, tag + "LNG"], w=[ok, tag + "xt%d" % i2])
        op("pool", lambda h: h.tensor_tensor(out=o[:], in0=o[:], in1=LNB[:], op=ALU.add),
           r=[ok, tag + "LNB"], w=[ok, tag + "xt%d" % i2])
        ev = dma("pool", x_dst[t * 128:(t + 1) * 128, :], o[:], r=[ok, tag + "xt%d" % i2], w=[(tag + "dst", t)])
        if C.get("_final"):
            k.out_events.append(ev)


def ffn(k, C, layer, x_src, x_dst, is_final):
    nc, op, dma, ps, sb = k.nc, k.op, k.dma, k.ps, k.sb
    ls = layer * 2 + 1
    T = "f%d" % layer
    utag = T + "UT"
    moe = (layer % 2 == 1)
    C["_final"] = is_final
    with ExitStack() as fs:
        X = sb(fs, "X", [128, NT, D], F32)
        UT = sb(fs, "UT", [128, 8, S], BF16)
        HT = sb(fs, "HT", [128, 4, S], BF16)
        WG = [sb(fs, "WG", [128, 8, 512], BF16) for _ in range(2)]
        WU = [sb(fs, "WU", [128, 8, 512], BF16) for _ in range(2)]
        WD = [sb(fs, "WD", [128, 4, D], BF16) for _ in range(2)]
        SGb = [sb(fs, "SGb", [128, 512], F32) for _ in range(2)]
        for g in range(4):
            dma("sp", X[:, 4 * g:4 * g + 4, :], x_src[g * 512:(g + 1) * 512, :].rearrange("(t p) d -> p t d", p=128),
                w=[("X", 4 * g + t, hf) for t in range(4) for hf in range(2)])
        router = None
        if moe:
            WR = sb(fs, "WR", [128, 8, NE], F32)
            LG = sb(fs, "LG", [128, NT, NE], F32)
            L2 = sb(fs, "L2", [128, NT, NE], F32)
            EQ1 = sb(fs, "EQ1", [128, NT, NE], F32)
            EQ2 = sb(fs, "EQ2", [128, NT, NE], F32)
            COMB = sb(fs, "COMB", [128, NT, NE], F32)
            m1 = sb(fs, "m1", [128, NT], F32)
            m2 = sb(fs, "m2", [128, NT], F32)
            w1 = sb(fs, "w1", [128, NT], F32)
            w2 = sb(fs, "w2", [128, NT], F32)
            us = ExitStack()
            UF = sb(us, "UF", [128, 8, 512], F32)
            with nc.allow_non_contiguous_dma(reason="tiny router weight"):
                dma("sp", WR[:], C["mr_in"].rearrange("(k p) e -> p k e", p=128), w=["WR"])

            def router(g, kk, pb, pk):
                op("act", lambda h: h.activation(out=UF[:, kk, :], in_=pb[:, :], func=AF.Identity,
                                                 scale=C["adac"][:, ls, 8 + kk:9 + kk],
                                                 bias=C["adac"][:, ls, kk:kk + 1]),
                   r=[pk, "adac"], w=[("UF", kk)])
                if kk == 7:
                    for t in range(4):
                        for k2 in range(8):
                            op("pe", lambda h: h.matmul(ps[2][:, t * 8:(t + 1) * 8],
                                                        lhsT=UF[:, k2, t * 128:(t + 1) * 128], rhs=WR[:, k2, :],
                                                        start=(k2 == 0), stop=(k2 == 7)),
                               r=[("UF", k2), "WR"], w=["ps2"])
                    op("dve", lambda h: h.tensor_copy(out=LG[:, 4 * g:4 * g + 4, :],
                                                      in_=ps[2][:, 0:32].rearrange("p (t e) -> p t e", e=NE)),
                       r=["ps2"], w=[("LG", g)])
        build_ut(k, C, UT, utag, ls, X=X, router=router)
        if moe:
            k.barrier()
            us.close()
        for t in range(NT):
            op("dve", lambda h: h.tensor_scalar(out=X[:, t, :], in0=X[:, t, :], scalar1=ALPHA, scalar2=None,
                                                op0=ALU.mult), r=[("X", t, 0), ("X", t, 1)], w=[("X", t, 0), ("X", t, 1)])
        grow = sb(fs, "growf", [128, D], F32)
        growb = sb(fs, "growb", [128, D], BF16)
        dma("sp", grow[:], C["grow_d"][ls], w=["growf"])
        op("dve", lambda h: h.tensor_copy(out=growb[:], in_=grow[:]), r=["growf"], w=["growb"])
        if moe:
            lgk = [("LG", g) for g in range(4)]
            bc = lambda a: a[:].unsqueeze(2).to_broadcast([128, NT, NE])
            op("dve", lambda h: h.tensor_reduce(out=m1[:], in_=LG[:], axis=AX.X, op=ALU.max), r=lgk, w=["m1"])
            op("dve", lambda h: h.tensor_tensor(out=EQ1[:], in0=LG[:], in1=bc(m1), op=ALU.is_equal),
               r=lgk + ["m1"], w=["EQ1"])
            op("dve", lambda h: h.scalar_tensor_tensor(out=L2[:], in0=EQ1[:], scalar=-1e30, in1=LG[:],
                                                        op0=ALU.mult, op1=ALU.add), r=lgk + ["EQ1"], w=["L2"])
            op("dve", lambda h: h.tensor_reduce(out=m2[:], in_=L2[:], axis=AX.X, op=ALU.max), r=["L2"], w=["m2"])
            op("dve", lambda h: h.tensor_tensor(out=EQ2[:], in0=L2[:], in1=bc(m2), op=ALU.is_equal),
               r=["L2", "m2"], w=["EQ2"])
            op("dve", lambda h: h.tensor_tensor(out=w2[:], in0=m1[:], in1=m2[:], op=ALU.subtract),
               r=["m1", "m2"], w=["w2"])
            op("act", lambda h: h.activation(out=w1[:], in_=w2[:], func=AF.Sigmoid), r=["w2"], w=["w1"])
            op("dve", lambda h: h.tensor_scalar(out=w2[:], in0=w1[:], scalar1=-1.0, scalar2=1.0, op0=ALU.mult,
                                                 op1=ALU.add), r=["w1", "w2"], w=["w2"])
            op("dve", lambda h: h.tensor_tensor(out=COMB[:], in0=EQ1[:], in1=bc(w1), op=ALU.mult),
               r=["EQ1", "w1"], w=["COMB"])
            op("dve", lambda h: h.tensor_tensor(out=EQ2[:], in0=EQ2[:], in1=bc(w2), op=ALU.mult),
               r=["EQ2", "w2"], w=["EQ2"])
            op("dve", lambda h: h.tensor_tensor(out=COMB[:], in0=COMB[:], in1=EQ2[:], op=ALU.add),
               r=["COMB", "EQ2"], w=["COMB"])
            k.dbg("COMB", COMB[:], [128, NT, NE], r=["COMB"])
        gi = 0
        it = 0
        ie = 0
        for e in (range(NE) if moe else [None]):
            dff = DFE if moe else DFF
            wg_src = C["mwg_in"][e] if moe else C["fwg_in"]
            wu_src = C["mwu_in"][e] if moe else C["fwu_in"]
            wd_src = C["mwd_in"][e] if moe else C["fwd_in"]
            wgv = wg_src.rearrange("(k p) f -> p k f", p=128)
            wuv = wu_src.rearrange("(k p) f -> p k f", p=128)
            for g in range((dff + 511) // 512):
                f0 = g * 512
                fw = min(512, dff - f0)
                chunks = [(c0, min(128, fw - c0)) for c0 in range(0, fw, 128)]
                b = gi % 2
                gi += 1
                dma("pool", WG[b][:, :, 0:fw], wgv[:, :, f0:f0 + fw], w=[("WG", b)])
                dma("pool", WU[b][:, :, 0:fw], wuv[:, :, f0:f0 + fw], w=[("WU", b)])
                for j, (c0, cs) in enumerate(chunks):
                    dma("pool", WD[b][0:cs, j, :], wd_src[f0 + c0:f0 + c0 + cs, :], w=[("WD", b, j)])
                for j, (c0, cs) in enumerate(chunks):
                    op("pool", lambda h: h.tensor_tensor(out=WD[b][0:cs, j, :], in0=WD[b][0:cs, j, :],
                                                         in1=growb[0:cs, :], op=ALU.mult),
                       r=["growb"], w=[("WD", b, j)])
                for j, (c0, cs) in enumerate(chunks):
                    for tb in range(4):
                        pg = (it % 2) * 2
                        pu = pg + 1
                        i2 = it % 2
                        it += 1
                        for kk in range(8):
                            op("pe", lambda h: h.matmul(ps[pg][0:cs, :], lhsT=WG[b][:, kk, c0:c0 + cs],
                                                        rhs=UT[:, kk, tb * 512:(tb + 1) * 512],
                                                        start=(kk == 0), stop=(kk == 7)),
                               r=[("WG", b), (utag, kk, tb)], w=["ps%d" % pg])
                        for kk in range(8):
                            op("pe", lambda h: h.matmul(ps[pu][0:cs, :], lhsT=WU[b][:, kk, c0:c0 + cs],
                                                        rhs=UT[:, kk, tb * 512:(tb + 1) * 512],
                                                        start=(kk == 0), stop=(kk == 7)),
                               r=[("WU", b), (utag, kk, tb)], w=["ps%d" % pu])
                        op("act", lambda h: h.activation(out=SGb[i2][0:cs, :], in_=ps[pg][0:cs, :], func=AF.Silu),
                           r=["ps%d" % pg], w=["SGb%d" % i2])
                        op("dve", lambda h: h.tensor_tensor(out=HT[0:cs, j, tb * 512:(tb + 1) * 512],
                                                             in0=ps[pu][0:cs, :], in1=SGb[i2][0:cs, :], op=ALU.mult),
                           r=["ps%d" % pu, "SGb%d" % i2], w=[("HT", j, tb)])
                for t in range(NT):
                    for half in range(2):
                        bank = 4 + (ie % 4)
                        i2 = ie % 2
                        ie += 1
                        bkey = "ps%d" % bank
                        for j, (c0, cs) in enumerate(chunks):
                            op("pe", lambda h: h.matmul(ps[bank][:, :], lhsT=HT[0:cs, j, t * 128:(t + 1) * 128],
                                                        rhs=WD[b][0:cs, j, half * 512:(half + 1) * 512],
                                                        start=(j == 0), stop=(j == len(chunks) - 1)),
                               r=[("HT", j, t // 4), ("WD", b, j)], w=[bkey])
                        sc = COMB[:, t, e:e + 1] if moe else 1.0
                        op("dve", lambda h: h.scalar_tensor_tensor(out=X[:, t, half * 512:(half + 1) * 512],
                                                                    in0=ps[bank][:, :], scalar=sc,
                                                                    in1=X[:, t, half * 512:(half + 1) * 512],
                                                                    op0=ALU.mult, op1=ALU.add),
                           r=[bkey, ("X", t, half)] + (["COMB"] if moe else []), w=[("X", t, half)])
        if layer == 0:
            pass
        resid_ln(k, C, fs, ls, None, x_src=None, x_dst=x_dst, tag=T + "o", X=X)
        k.barrier()
```
